# Optimizing a Trainium2 kernel written in Bass

```python
import math
import jax
import jax.numpy as jnp
from jax import lax
import numpy as np

D_MODEL = 1024
BATCH = 2
SEQ = 8192
DEPTH = 2

CHUNK = 64
CONV_K = 4
GDN_HEADS = 4
GDN_DK = 128
GDN_DV = 128
MLSTM_HEADS = 4
MLSTM_DQK = 64
MLSTM_DV = 128
MLSTM_SOFTCAP = 15.0
RWKV_HEADS = 8
RWKV_DH = 64
RWKV_WIDTH = RWKV_HEADS * RWKV_DH
LORA_W = 64
LORA_A = 64
LORA_V = 32
LORA_G = 128
N_BRANCH = 3
BRANCH_WIDTH = 512
D_FF = 2816
N_EXPERTS = 8
TOP_K = 2
D_FF_EXPERT = 3584
MOE_BLOCK = 256
DEEPNORM_ALPHA = (2 * DEPTH) ** 0.25
DEEPNORM_BETA = (8 * DEPTH) ** -0.25
LN_EPS = 1e-5
RWKV_GN_EPS = 64e-5

GDN_SIZES = (GDN_HEADS * GDN_DK, GDN_HEADS * GDN_DK, GDN_HEADS * GDN_DV, GDN_HEADS * GDN_DV, GDN_HEADS, GDN_HEADS)
MLSTM_SIZES = (MLSTM_HEADS * MLSTM_DQK, MLSTM_HEADS * MLSTM_DQK, MLSTM_HEADS * MLSTM_DV, MLSTM_HEADS * MLSTM_DV, MLSTM_HEADS, MLSTM_HEADS)
RWKV_SIZES = (RWKV_WIDTH, RWKV_WIDTH, RWKV_WIDTH, LORA_W, LORA_A, LORA_G)
GATE_SIZES = (D_MODEL,) * N_BRANCH
IN_GROUPS = (sum(GDN_SIZES), sum(MLSTM_SIZES), sum(RWKV_SIZES), sum(GATE_SIZES))
IN_WIDTH = sum(IN_GROUPS)

kernel_name = 'hybrid_gdn_mlstm_rwkv7_moe_deepnorm'


def _split(z, sizes):
    return jnp.split(z, np.cumsum(sizes)[:-1].tolist(), axis=-1)


def _layer_norm(x, g, b):
    xf = x.astype(jnp.float32)
    mu = jnp.mean(xf, -1, keepdims=True)
    var = jnp.mean(jnp.square(xf - mu), -1, keepdims=True)
    return ((xf - mu) * lax.rsqrt(var + LN_EPS)).astype(x.dtype) * g + b


def _head_split(z, n_heads):
    return z.reshape(z.shape[:-1] + (n_heads, z.shape[-1] // n_heads))


def _head_rms(z, n_heads, eps=1e-6):
    zh = _head_split(z, n_heads)
    return zh * lax.rsqrt(jnp.mean(zh * zh, -1, keepdims=True) + eps)


def _l2norm(z, eps=1e-6):
    return z * lax.rsqrt(jnp.sum(z * z, -1, keepdims=True) + eps)


def _to_chunks(z, n_heads):
    b, t, hd = z.shape
    return z.reshape(b, t // CHUNK, CHUNK, n_heads, hd // n_heads).transpose(0, 3, 1, 2, 4)


def _scalar_chunks(s):
    b, t, h = s.shape
    return s.reshape(b, t // CHUNK, CHUNK, h).transpose(0, 3, 1, 2)


def _from_chunks(z):
    b, h, nc, c, d = z.shape
    return z.transpose(0, 2, 3, 1, 4).reshape(b, nc * c, h * d)


def _causal_masks():
    incl = jnp.tril(jnp.ones((CHUNK, CHUNK), bool))
    return incl, jnp.tril(incl, -1)


def _unit_lower_inverse(a):
    eye = jnp.broadcast_to(jnp.eye(CHUNK, dtype=a.dtype), a.shape)
    return lax.linalg.triangular_solve(eye + a, eye, left_side=True, lower=True, unit_diagonal=True)


def _chunk_scan(step, init, xs):
    _, ys = lax.scan(step, init, tuple(jnp.moveaxis(t, 2, 0) for t in xs))
    return jnp.moveaxis(ys, 0, 2)


def _causal_conv(z, w):
    return lax.conv_general_dilated(z, w[:, None, :].astype(z.dtype), window_strides=(1,), padding=((CONV_K - 1, 0),),
                                    dimension_numbers=('NWC', 'WIO', 'NWC'), feature_group_count=z.shape[-1])


def _token_shift(z, mu):
    prev = jnp.pad(z, ((0, 0), (1, 0), (0, 0)))[:, :-1]
    return z + mu * (prev - z)


def _swiglu(h, w_gate, w_up, w_down):
    return (jax.nn.silu(h @ w_gate) * (h @ w_up)) @ w_down


def _gated_deltanet(q, k, v, z, a, b, conv_w, a_log, dt_bias, norm_g):
    f32 = jnp.float32
    hh = GDN_HEADS
    qkv = jax.nn.silu(_causal_conv(jnp.concatenate([q, k, v], -1), conv_w)).astype(f32)
    q, k, v = _split(qkv, GDN_SIZES[:3])
    q = _l2norm(_to_chunks(q, hh)) * GDN_DK ** -0.5
    k = _l2norm(_to_chunks(k, hh))
    v = _to_chunks(v, hh)
    g = _scalar_chunks(-jnp.exp(a_log.astype(f32)) * jax.nn.softplus(a.astype(f32) + dt_bias.astype(f32)))
    beta = _scalar_chunks(jax.nn.sigmoid(b.astype(f32)))
    g = jnp.cumsum(g, -1)
    incl, strict = _causal_masks()
    decay = jnp.exp(jnp.where(incl, g[..., :, None] - g[..., None, :], -jnp.inf))
    kb = k * beta[..., None]
    a_kk = jnp.where(strict, jnp.einsum('bhnck,bhnsk->bhncs', kb, k) * decay, 0.0)
    t_inv = _unit_lower_inverse(a_kk)
    u = jnp.einsum('bhncs,bhnsv->bhncv', t_inv, v * beta[..., None])
    w = jnp.einsum('bhncs,bhnsk->bhnck', t_inv, kb * jnp.exp(g)[..., None])
    qk = jnp.einsum('bhnck,bhnsk->bhncs', q, k) * decay
    q_dec = q * jnp.exp(g)[..., None]
    k_dec = k * jnp.exp(g[..., -1:] - g)[..., None]
    g_last = jnp.exp(g[..., -1])

    def step(s, inp):
        qd, wc, uc, qkc, kd, gl = inp
        v_new = uc - jnp.einsum('bhck,bhkv->bhcv', wc, s)
        o = jnp.einsum('bhck,bhkv->bhcv', qd, s) + jnp.einsum('bhcs,bhsv->bhcv', qkc, v_new)
        s = s * gl[..., None, None] + jnp.einsum('bhck,bhcv->bhkv', kd, v_new)
        return s, o

    s0 = jnp.zeros(q.shape[:2] + (GDN_DK, GDN_DV), f32)
    o = _chunk_scan(step, s0, (q_dec, w, u, qk, k_dec, g_last))
    o = _head_rms(_from_chunks(o), hh) * norm_g.astype(f32)
    return (o.reshape(o.shape[:2] + (-1,)) * jax.nn.silu(z.astype(f32))).astype(z.dtype)


def _mlstm(q, k, v, o_pre, i_pre, f_pre, b_i, b_f, norm_g):
    f32 = jnp.float32
    hh = MLSTM_HEADS
    q = _to_chunks(q.astype(f32), hh)
    k = _to_chunks(k.astype(f32), hh) * MLSTM_DQK ** -0.5
    v = _to_chunks(v.astype(f32), hh)
    cap = lambda t: MLSTM_SOFTCAP * jnp.tanh(t / MLSTM_SOFTCAP)
    log_i = _scalar_chunks(cap(i_pre.astype(f32) + b_i.astype(f32)))
    log_f = _scalar_chunks(jax.nn.log_sigmoid(cap(f_pre.astype(f32) + b_f.astype(f32))))
    bcum = jnp.cumsum(log_f, -1)
    incl, _ = _causal_masks()
    log_d = jnp.where(incl, bcum[..., :, None] - bcum[..., None, :] + log_i[..., None, :], -jnp.inf)
    m_intra = jnp.max(log_d, -1)
    p = jnp.exp(log_d - m_intra[..., None]) * jnp.einsum('bhnck,bhnsk->bhncs', q, k)
    num_intra = jnp.einsum('bhncs,bhnsv->bhncv', p, v)
    den_intra = jnp.sum(p, -1)
    log_e = bcum[..., -1:] - bcum + log_i
    m_end = jnp.max(log_e, -1)
    k_w = k * jnp.exp(log_e - m_end[..., None])[..., None]
    b_last = bcum[..., -1]

    def step(carry, inp):
        cs, ns, m = carry
        qc, bc, mi, numi, deni, kw, vc, me, bl = inp
        m_t = jnp.maximum(bc + m[..., None], mi)
        s_inter = jnp.exp(bc + m[..., None] - m_t)
        s_intra = jnp.exp(mi - m_t)
        num = s_inter[..., None] * jnp.einsum('bhck,bhkv->bhcv', qc, cs) + s_intra[..., None] * numi
        den = s_inter * jnp.einsum('bhck,bhk->bhc', qc, ns) + s_intra * deni
        hc = num / jnp.maximum(jnp.abs(den), jnp.exp(-m_t))[..., None]
        m_new = jnp.maximum(bl + m, me)
        s_old = jnp.exp(bl + m - m_new)
        s_new = jnp.exp(me - m_new)
        cs = s_old[..., None, None] * cs + s_new[..., None, None] * jnp.einsum('bhck,bhcv->bhkv', kw, vc)
        ns = s_old[..., None] * ns + s_new[..., None] * jnp.sum(kw, -2)
        return (cs, ns, m_new), hc

    bh = q.shape[:2]
    init = (jnp.zeros(bh + (MLSTM_DQK, MLSTM_DV), f32), jnp.zeros(bh + (MLSTM_DQK,), f32), jnp.zeros(bh, f32))
    hs = _chunk_scan(step, init, (q, bcum, m_intra, num_intra, den_intra, k_w, v, m_end, b_last))
    hs = _head_rms(_from_chunks(hs), hh) * norm_g.astype(f32).reshape(hh, MLSTM_DV)
    return (hs.reshape(hs.shape[:2] + (-1,)) * jax.nn.sigmoid(o_pre.astype(f32))).astype(o_pre.dtype)


def _rwkv7(r, k, v, x_w, x_a, x_g, w0, w2, a0, a2, g2, k_k, k_a, r_k, lnx_g, lnx_b, v_first, v_mix):
    f32 = jnp.float32
    hh = RWKV_HEADS
    r, k, v = r.astype(f32), k.astype(f32), v.astype(f32)
    log_w = -jnp.exp(-jax.nn.softplus(-(w0.astype(f32) + jnp.tanh(x_w.astype(f32)) @ w2.astype(f32))) - 0.5)
    a = jax.nn.sigmoid(a0.astype(f32) + x_a.astype(f32) @ a2.astype(f32))
    if v_mix is None:
        v_first = v
    else:
        x_v, v0, v2 = v_mix
        v = v + (v_first - v) * jax.nn.sigmoid(v0.astype(f32) + x_v.astype(f32) @ v2.astype(f32))
    g = jax.nn.sigmoid(x_g.astype(f32)) @ g2.astype(f32)
    kk = _l2norm(_head_split(k * k_k.astype(f32), hh)).reshape(k.shape)
    k = k * (1.0 + (a - 1.0) * k_a.astype(f32))
    rc, kc, vc, kkc, ac, lwc = (_to_chunks(t, hh) for t in (r, k, v, kk, a, log_w))
    alpha, beta = -kkc, kkc * ac
    lw = jnp.cumsum(lwc, 3)
    lw_ex = lw - lwc
    lw_last = lw[..., -1:, :]
    inv = jnp.exp(-lw)
    al_bar = alpha * jnp.exp(lw_ex)
    be_hat, k_hat = beta * inv, kc * inv
    r_bar = rc * jnp.exp(lw)
    incl, strict = _causal_masks()
    dots = lambda p, q: jnp.einsum('bhnck,bhnsk->bhncs', p, q)
    mix = lambda m, z: jnp.einsum('bhncs,bhnsd->bhncd', m, z)
    l_ab = jnp.where(strict, dots(al_bar, be_hat), 0.0)
    l_ak = jnp.where(strict, dots(al_bar, k_hat), 0.0)
    q_rb = jnp.where(incl, dots(r_bar, be_hat), 0.0)
    q_rk = jnp.where(incl, dots(r_bar, k_hat), 0.0)
    t_inv = _unit_lower_inverse(-l_ab)
    w_u = mix(t_inv, al_bar)
    u0 = mix(t_inv, mix(l_ak, vc))
    y_k = mix(q_rk, vc)
    to_end = jnp.exp(lw_last - lw)
    be_end, k_end = beta * to_end, kc * to_end
    g_chunk = jnp.exp(lw_last[..., 0, :])

    def step(s, inp):
        wu, u0c, rb, qrb, ykc, bec, kec, vcc, gc = inp
        u = jnp.einsum('bhck,bhkv->bhcv', wu, s) + u0c
        y = jnp.einsum('bhck,bhkv->bhcv', rb, s) + jnp.einsum('bhcs,bhsv->bhcv', qrb, u) + ykc
        s = gc[..., :, None] * s + jnp.einsum('bhck,bhcv->bhkv', bec, u) + jnp.einsum('bhck,bhcv->bhkv', kec, vcc)
        return s, y

    s0 = jnp.zeros(rc.shape[:2] + (RWKV_DH, RWKV_DH), f32)
    y = _from_chunks(_chunk_scan(step, s0, (w_u, u0, r_bar, q_rb, y_k, be_end, k_end, vc, g_chunk)))
    yh = _head_split(y, hh)
    mu = jnp.mean(yh, -1, keepdims=True)
    var = jnp.mean(jnp.square(yh - mu), -1, keepdims=True)
    y = ((yh - mu) * lax.rsqrt(var + RWKV_GN_EPS)).reshape(y.shape) * lnx_g.astype(f32) + lnx_b.astype(f32)
    bonus = jnp.sum(_head_split(r * k * r_k.astype(f32).reshape(-1), hh), -1, keepdims=True) * _head_split(v, hh)
    y = (y + bonus.reshape(y.shape)) * g
    return y.astype(x_w.dtype), v_first


def _mixer_sublayer(h, w_in, gdn_conv, gdn_a_log, gdn_dt_bias, gdn_norm_g, mlstm_b_i, mlstm_b_f, mlstm_norm_g,
                    rwkv_mu, rwkv_w0, rwkv_w2, rwkv_a0, rwkv_a2, rwkv_g2, rwkv_k_k, rwkv_k_a, rwkv_r_k,
                    rwkv_lnx_g, rwkv_lnx_b, w_branch, w_o, v_first, v_res):
    w = w_in if v_res is None else jnp.concatenate([w_in, v_res[0]], axis=1)
    z_all = h @ w
    z, z_v = z_all[..., :IN_WIDTH], z_all[..., IN_WIDTH:]
    z_gdn, z_mlstm, z_rwkv, z_gate = _split(z, IN_GROUPS)
    gq, gk, gv, gz, ga, gb = _split(z_gdn, GDN_SIZES)
    y_a = _gated_deltanet(gq, gk, gv, gz, ga, gb, gdn_conv, gdn_a_log, gdn_dt_bias, gdn_norm_g)
    mq, mk, mv, mo, mi, mf = _split(z_mlstm, MLSTM_SIZES)
    y_b = _mlstm(mq, mk, mv, mo, mi, mf, mlstm_b_i, mlstm_b_f, mlstm_norm_g)
    rr, rk, rv, rw, ra, rg = _split(_token_shift(z_rwkv, rwkv_mu), RWKV_SIZES)
    v_mix = None if v_res is None else (_token_shift(z_v, v_res[1]), v_res[2], v_res[3])
    y_c, v_first = _rwkv7(rr, rk, rv, rw, ra, rg, rwkv_w0, rwkv_w2, rwkv_a0, rwkv_a2, rwkv_g2, rwkv_k_k, rwkv_k_a,
                          rwkv_r_k, rwkv_lnx_g, rwkv_lnx_b, v_first, v_mix)
    gate_a, gate_b, gate_c = _split(jax.nn.sigmoid(z_gate), GATE_SIZES)
    merged = gate_a * (y_a @ w_branch[0]) + gate_b * (y_b @ w_branch[1]) + gate_c * (y_c @ w_branch[2])
    return merged @ w_o, v_first


def _moe_swiglu(h, router_w, router_b, w_gate, w_up, w_down):
    b, t, d = h.shape
    n = b * t
    xf = h.reshape(n, d)
    logits = xf.astype(jnp.float32) @ router_w.astype(jnp.float32) + router_b.astype(jnp.float32)
    top_logit, top_e = lax.top_k(logits, TOP_K)
    gate = jax.nn.softmax(top_logit, axis=-1)
    n_assign = n * TOP_K
    e_flat = top_e.reshape(-1)
    tok_flat = jnp.arange(n_assign, dtype=jnp.int32) // TOP_K
    order = jnp.argsort(e_flat)
    e_sorted = e_flat[order]
    counts = jnp.zeros((N_EXPERTS,), jnp.int32).at[e_flat].add(1)
    padded = (counts + MOE_BLOCK - 1) // MOE_BLOCK * MOE_BLOCK
    pad_end = jnp.cumsum(padded)
    pad_start = pad_end - padded
    start = jnp.cumsum(counts) - counts
    dest = pad_start[e_sorted] + jnp.arange(n_assign, dtype=jnp.int32) - start[e_sorted]
    n_blocks = -(-n_assign // MOE_BLOCK) + N_EXPERTS
    rows = n_blocks * MOE_BLOCK
    row_tok = jnp.full((rows,), n, jnp.int32).at[dest].set(tok_flat[order])
    row_gate = jnp.zeros((rows,), h.dtype).at[dest].set(gate.reshape(-1)[order].astype(h.dtype))
    block_e = jnp.minimum(jnp.searchsorted(pad_end, jnp.arange(n_blocks, dtype=jnp.int32) * MOE_BLOCK, side='right'), N_EXPERTS - 1)
    x_pad = jnp.concatenate([xf, jnp.zeros((1, d), xf.dtype)], 0)
    xb = x_pad[row_tok].reshape(n_blocks, MOE_BLOCK, d)

    def expert_block(args):
        xblk, e = args
        return _swiglu(xblk, w_gate[e], w_up[e], w_down[e])

    yb = lax.map(expert_block, (xb, block_e)).reshape(rows, d)
    y = jnp.zeros((n + 1, d), h.dtype).at[row_tok].add(yb * row_gate[:, None])
    return y[:n].reshape(b, t, d)


def setup_inputs(seed: int = 0) -> dict:
    key = jax.random.key(seed)
    keys = iter(jax.random.split(key, 64))
    f32 = jnp.float32

    def normal(shape, scale):
        return jax.random.normal(next(keys), shape, f32) * scale

    def uniform(shape, lo, hi):
        return jax.random.uniform(next(keys), shape, f32, lo, hi)

    d = D_MODEL
    n_even, n_odd, n_res = (DEPTH + 1) // 2, DEPTH // 2, DEPTH - 1
    dt = jnp.exp(uniform((DEPTH, GDN_HEADS), math.log(1e-3), math.log(1e-1)))
    return {
        'x': normal((BATCH, SEQ, d), 1.0),
        'c': normal((BATCH, d), 1.0),
        'ln_in_g': 1.0 + normal((d,), 0.02),
        'ln_in_b': normal((d,), 0.02),
        'ada_w': normal((DEPTH, d, 6 * d), 0.3 * d ** -0.5),
        'ada_b': normal((DEPTH, 6 * d), 0.02),
        'w_in': normal((DEPTH, d, IN_WIDTH), d ** -0.5),
        'gdn_conv': normal((DEPTH, CONV_K, sum(GDN_SIZES[:3])), CONV_K ** -0.5),
        'gdn_a_log': jnp.log(uniform((DEPTH, GDN_HEADS), 1.0, 16.0)),
        'gdn_dt_bias': dt + jnp.log(-jnp.expm1(-dt)),
        'gdn_norm_g': 1.0 + normal((DEPTH, GDN_DV), 0.02),
        'mlstm_b_i': normal((DEPTH, MLSTM_HEADS), 0.1),
        'mlstm_b_f': jnp.linspace(3.0, 6.0, MLSTM_HEADS, dtype=f32) + normal((DEPTH, MLSTM_HEADS), 0.1),
        'mlstm_norm_g': 1.0 + normal((DEPTH, MLSTM_HEADS * MLSTM_DV), 0.02),
        'rwkv_mu': uniform((DEPTH, sum(RWKV_SIZES)), 0.0, 1.0),
        'rwkv_w0': uniform((DEPTH, RWKV_WIDTH), -6.0, -1.0),
        'rwkv_w2': normal((DEPTH, LORA_W, RWKV_WIDTH), 0.1 * LORA_W ** -0.5),
        'rwkv_a0': normal((DEPTH, RWKV_WIDTH), 0.1),
        'rwkv_a2': normal((DEPTH, LORA_A, RWKV_WIDTH), 0.1 * LORA_A ** -0.5),
        'rwkv_g2': normal((DEPTH, LORA_G, RWKV_WIDTH), LORA_G ** -0.5),
        'rwkv_k_k': 0.85 + normal((DEPTH, RWKV_WIDTH), 0.05),
        'rwkv_k_a': 1.0 + normal((DEPTH, RWKV_WIDTH), 0.05),
        'rwkv_r_k': normal((DEPTH, RWKV_HEADS, RWKV_DH), 0.1),
        'rwkv_lnx_g': 1.0 + normal((DEPTH, RWKV_WIDTH), 0.02),
        'rwkv_lnx_b': normal((DEPTH, RWKV_WIDTH), 0.02),
        'rwkv_v1': normal((n_res, d, LORA_V), d ** -0.5),
        'rwkv_mu_v1': uniform((n_res, LORA_V), 0.0, 1.0),
        'rwkv_v0': 1.0 + normal((n_res, RWKV_WIDTH), 0.1),
        'rwkv_v2': normal((n_res, LORA_V, RWKV_WIDTH), 0.1 * LORA_V ** -0.5),
        'w_branch': normal((DEPTH, N_BRANCH, BRANCH_WIDTH, d), DEEPNORM_BETA * BRANCH_WIDTH ** -0.5),
        'w_o': normal((DEPTH, d, d), DEEPNORM_BETA * d ** -0.5),
        'ln1_g': 1.0 + normal((DEPTH, d), 0.02),
        'ln1_b': normal((DEPTH, d), 0.02),
        'ln2_g': 1.0 + normal((DEPTH, d), 0.02),
        'ln2_b': normal((DEPTH, d), 0.02),
        'ffn_w_gate': normal((n_even, d, D_FF), d ** -0.5),
        'ffn_w_up': normal((n_even, d, D_FF), d ** -0.5),
        'ffn_w_down': normal((n_even, D_FF, d), DEEPNORM_BETA * D_FF ** -0.5),
        'moe_router': normal((n_odd, d, N_EXPERTS), d ** -0.5),
        'moe_router_b': normal((n_odd, N_EXPERTS), 0.01),
        'moe_w_gate': normal((n_odd, N_EXPERTS, d, D_FF_EXPERT), d ** -0.5),
        'moe_w_up': normal((n_odd, N_EXPERTS, d, D_FF_EXPERT), d ** -0.5),
        'moe_w_down': normal((n_odd, N_EXPERTS, D_FF_EXPERT, d), DEEPNORM_BETA * D_FF_EXPERT ** -0.5),
    }


def reference(x, c, ln_in_g, ln_in_b, ada_w, ada_b, w_in, gdn_conv, gdn_a_log, gdn_dt_bias, gdn_norm_g,
              mlstm_b_i, mlstm_b_f, mlstm_norm_g, rwkv_mu, rwkv_w0, rwkv_w2, rwkv_a0, rwkv_a2, rwkv_g2,
              rwkv_k_k, rwkv_k_a, rwkv_r_k, rwkv_lnx_g, rwkv_lnx_b, rwkv_v1, rwkv_mu_v1, rwkv_v0, rwkv_v2,
              w_branch, w_o, ln1_g, ln1_b, ln2_g, ln2_b, ffn_w_gate, ffn_w_up, ffn_w_down,
              moe_router, moe_router_b, moe_w_gate, moe_w_up, moe_w_down):
    x = _layer_norm(x, ln_in_g, ln_in_b)
    cond = jax.nn.silu(c)
    v_first = None
    for layer in range(DEPTH):
        mod = cond @ ada_w[layer] + ada_b[layer]
        sh1, sc1, gt1, sh2, sc2, gt2 = jnp.split(mod[:, None, :], 6, axis=-1)
        v_res = None if layer == 0 else (rwkv_v1[layer - 1], rwkv_mu_v1[layer - 1], rwkv_v0[layer - 1], rwkv_v2[layer - 1])
        y, v_first = _mixer_sublayer(x * (1.0 + sc1) + sh1, w_in[layer], gdn_conv[layer], gdn_a_log[layer],
                                     gdn_dt_bias[layer], gdn_norm_g[layer], mlstm_b_i[layer], mlstm_b_f[layer],
                                     mlstm_norm_g[layer], rwkv_mu[layer], rwkv_w0[layer], rwkv_w2[layer],
                                     rwkv_a0[layer], rwkv_a2[layer], rwkv_g2[layer], rwkv_k_k[layer],
                                     rwkv_k_a[layer], rwkv_r_k[layer], rwkv_lnx_g[layer], rwkv_lnx_b[layer],
                                     w_branch[layer], w_o[layer], v_first, v_res)
        x = _layer_norm(DEEPNORM_ALPHA * x + (1.0 + gt1) * y, ln1_g[layer], ln1_b[layer])
        h = x * (1.0 + sc2) + sh2
        if layer % 2 == 0:
            i = layer // 2
            y = _swiglu(h, ffn_w_gate[i], ffn_w_up[i], ffn_w_down[i])
        else:
            i = layer // 2
            y = _moe_swiglu(h, moe_router[i], moe_router_b[i], moe_w_gate[i], moe_w_up[i], moe_w_down[i])
        x = _layer_norm(DEEPNORM_ALPHA * x + (1.0 + gt2) * y, ln2_g[layer], ln2_b[layer])
    return x
```

```python
import contextlib
import numpy as np
import concourse.bass as bass
import concourse.mybir as mybir
from concourse.bass_utils import run_bass_kernel_spmd

F32 = mybir.dt.float32
BF16 = mybir.dt.bfloat16
AF = mybir.ActivationFunctionType
ALU = mybir.AluOpType
AX = mybir.AxisListType

ENGS = ("pe", "act", "dve", "pool", "sp")


class Tok:
    __slots__ = ("w", "r", "dsem", "dcnt", "name", "isout", "isdram", "wl", "excl")

    def __init__(self, name=""):
        self.excl = False
        self.isdram = False
        self.wl = []
        self.w = None
        self.r = []
        self.dsem = None
        self.dcnt = 0
        self.name = name
        self.isout = False


class V:
    __slots__ = ("ap", "toks")

    def __init__(self, ap, toks):
        self.ap = ap
        self.toks = toks

    def __getitem__(self, idx):
        return V(self.ap[idx], self.toks)

    def re(self, pat, **kw):
        return V(self.ap.rearrange(pat, **kw), self.toks)

    def pb(self, n):
        return V(self.ap.partition_broadcast(n), self.toks)

    def bt(self, shape):
        return V(self.ap.broadcast_to(list(shape)), self.toks)

    def tb(self, shape):
        return V(self.ap.to_broadcast(list(shape)), self.toks)


class T:
    def __init__(self, handle, name):
        self.t = handle
        self.tok = Tok(name)

    def __getitem__(self, idx):
        return V(self.t[idx], [self.tok])

    def ap(self):
        return V(self.t.ap() if hasattr(self.t, "ap") and callable(getattr(self.t, "ap")) else self.t[:], [self.tok])


class Tsub:
    def __init__(self, base_ap, name, tok=None):
        self.base = base_ap
        self.tok = tok if tok is not None else Tok(name)

    def __getitem__(self, idx):
        return V(self.base[idx], [self.tok])


class Op:
    __slots__ = ("eng", "fn", "deps", "isdma", "dtok", "handle", "awaited", "idx", "outdram", "incval", "epoch")

    def __init__(self, eng, fn, isdma=False, dtok=None):
        self.eng = eng
        self.fn = fn
        self.deps = []
        self.isdma = isdma
        self.dtok = dtok
        self.handle = None
        self.awaited = False
        self.outdram = False
        self.incval = 16


def _nofn(eng):
    return None


def _need(op, d, raw):
    if (not op.isdma) and (not d.isdma) and op.eng == "pe" and d.eng == "pe" and not raw:
        return False
    return True


def _aps(x):
    return x.ap if isinstance(x, V) else x


class Prog:
    def __init__(self, nc):
        self.nc = nc
        self.es = contextlib.ExitStack()
        self.ops = {e: [] for e in ENGS}
        self.all_ops = []
        self.scopes = [self.es]
        self.dma_last = {}
        self.dynval = None
        self.arena = None
        self.in_scope = False
        self.arena_off = 0
        self.epoch = 0
        self.n = 0
        self.outtoks = []

    def sb(self, shape, dt=F32, name=None):
        self.n += 1
        name = name or f"sb{self.n}"
        if self.in_scope and dt in (F32, BF16):
            p = shape[0]
            n = int(np.prod(shape[1:]))
            nw = n if dt == F32 else (n + 1) // 2
            n2 = (nw + 7) // 8 * 8
            assert self.arena_off + n2 <= self.ARENA, f"arena overflow allocating {name} {shape}: off={self.arena_off}"
            v = self.arena[0:p, self.arena_off:self.arena_off + nw]
            if dt == BF16:
                v = v.bitcast(BF16)[:, 0:n]
            self.arena_off += n2
            if len(shape) == 3:
                v = v.rearrange("p (a b) -> p a b", a=shape[1], b=shape[2])
            elif len(shape) == 4:
                v = v.rearrange("p (a b c) -> p a b c", a=shape[1], b=shape[2], c=shape[3])
            return T(v, name)
        return T(self.es.enter_context(self.nc.sbuf_tensor(name, list(shape), dt)), name)

    def ps(self, shape, dt=F32, name=None):
        self.n += 1
        name = name or f"ps{self.n}"
        t = T(self.es.enter_context(self.nc.psum_tensor(name, list(shape), dt)), name)
        t.tok.excl = True
        return t

    def dram(self, name, shape, dt=F32, kind="Internal"):
        t = T(self.nc.dram_tensor(name, list(shape), dt, kind=kind), name)
        t.tok.isdram = True
        if kind == "ExternalOutput":
            t.tok.isout = True
            self.outtoks.append(t.tok)
        return t

    ARENA = 52000

    @contextlib.contextmanager
    def scope(self):
        if self.arena is None:
            self.arena = self.es.enter_context(self.nc.sbuf_tensor("arena", [128, self.ARENA], F32))
        self.in_scope = True
        self.arena_off = 0
        try:
            yield
        finally:
            self.barrier()
            self.in_scope = False

    def barrier(self):
        deps = []
        for e in ENGS:
            comp = [o for o in self.ops[e] if not o.isdma and o.fn is not _nofn]
            if comp:
                deps.append(comp[-1])
        deps += list(self.dma_last.values())
        for e in ENGS:
            op = Op(e, _nofn)
            op.deps = [(d, True) for d in deps]
            op.epoch = self.epoch
            self.ops[e].append(op)
            self.all_ops.append(op)
        self.epoch += 1

    def load_dyn(self, src):
        ap = src.ap

        def fn(e):
            reg = self.es.enter_context(e.register("dynreg"))
            ins = e.reg_load(reg, ap)
            self.dynval = e.snap(reg, min_val=0, max_val=3)
            return ins
        return self._rec(Op("sp", fn), [src], [])

    def _rec(self, op, reads, writes):
        if getattr(self, "defer", None) is not None:
            self.defer.append((op, list(reads), list(writes)))
            return op
        xr = []
        for v in reads:
            for tk in (v.toks if isinstance(v, V) else [v]):
                if tk.excl:
                    xr.append(tk)
                    continue
                w = tk.w
                if tk.isdram:
                    for w_ in tk.wl:
                        op.deps.append((w_, True))
                elif w is not None:
                    op.deps.append((w, True))
                if not op.isdma:
                    tk.r = [r for r in tk.r if r.isdma or r.eng != op.eng]
                tk.r.append(op)
        wl_ = [(tk, True) for tk in xr]
        for v in writes:
            for tk in (v.toks if isinstance(v, V) else [v]):
                wl_.append((tk, False))
        for tk, israw in wl_:
            if True:
                w = tk.w
                if w is not None and not (w.isdma and op.isdma):
                    op.deps.append((w, israw))
                for r in tk.r:
                    if r is not op:
                        op.deps.append((r, False))
                tk.r = []
                tk.w = op
                if tk.isdram:
                    tk.wl.append(op)
        op.epoch = self.epoch
        op.deps = [(d, raw) for d, raw in op.deps if d.epoch >= self.epoch]
        self.ops[op.eng].append(op)
        self.all_ops.append(op)
        if op.isdma:
            self.dma_last[id(op.dtok)] = op
        return op

    def op(self, eng, fn, reads=(), writes=()):
        return self._rec(Op(eng, fn), reads, writes)

    def begin_defer(self):
        self.defer = []

    def end_defer(self):
        d, self.defer = self.defer, None
        return d

    def merge(self, lists):
        def toks(vs):
            return [tk for v in vs for tk in (v.toks if isinstance(v, V) else [v])]
        touched, written = [], []
        for L in lists:
            t_, w_ = set(), set()
            for op, reads, writes in L:
                for tk in toks(reads):
                    if not tk.isdram:
                        t_.add(id(tk))
                for tk in toks(writes):
                    if not tk.isdram:
                        t_.add(id(tk)); w_.add(id(tk))
            touched.append(t_); written.append(w_)
        for a in range(len(lists)):
            for b in range(len(lists)):
                if a != b:
                    assert not (written[a] & touched[b]), "interleaved streams share a written tile"
        idx = [0] * len(lists)
        tot = [max(1, len(L)) for L in lists]
        while any(idx[i] < len(lists[i]) for i in range(len(lists))):
            i = min((k for k in range(len(lists)) if idx[k] < len(lists[k])), key=lambda k: idx[k] / tot[k])
            op, reads, writes = lists[i][idx[i]]
            idx[i] += 1
            self._rec(op, reads, writes)

    def dma(self, out, in_, q="sp", extra_reads=(), dyn=None):
        dtok = out.toks[0]
        if dtok.isdram:
            dtok = in_.toks[0]
            if dtok.isdram:
                self.n += 1
                dtok = Tok(f"dd{self.n}")
        o, i = out.ap, in_.ap
        if dyn is not None:
            op = Op(q, lambda e: e.dma_start(out=o, in_=dyn(self.dynval)), isdma=True, dtok=dtok)
        else:
            op = Op(q, lambda e: e.dma_start(out=o, in_=i), isdma=True, dtok=dtok)
        op.outdram = out.toks[0].isout
        return self._rec(op, [in_] + list(extra_reads), [out])

    def collective(self, kind, out, in_, groups):
        if getattr(self, "cct", None) is None:
            self.cct = Tok("cc")
        cct = self.cct
        o, i = out.ap, in_.ap
        op = Op("pool", lambda e: e.collective_compute(kind, ALU.bypass, replica_groups=groups, ins=[i], outs=[o]), isdma=True, dtok=cct)
        op.incval = 1
        return self._rec(op, [in_], [out])

    def all_gather_rows(self, dst, src, rc, groups):
        R = src.t.shape[0]
        for c in range(R // rc):
            self.collective("AllGather", dst[c].re("i r w -> (i r) w"), src[c * rc:(c + 1) * rc, :], groups)

    def mm(self, out, lhsT, rhs, start=True, stop=True, extra_reads=()):
        o, l, r = out.ap, lhsT.ap, rhs.ap
        return self.op("pe", lambda e: e.matmul(o, l, r, start=start, stop=stop), [lhsT, rhs] + list(extra_reads), [out])

    def tr(self, out, in_, ident):
        o, i, d = out.ap, in_.ap, ident.ap
        return self.op("pe", lambda e: e.transpose(o, i, d), [in_, ident], [out])

    def act(self, out, in_, func, bias=None, scale=None, accum=None, eng="act"):
        kw = {}
        reads = [in_]
        if bias is not None:
            kw["bias"] = _aps(bias)
            if isinstance(bias, V):
                reads.append(bias)
        if scale is not None:
            kw["scale"] = _aps(scale)
            if isinstance(scale, V):
                reads.append(scale)
        writes = [out]
        if accum is not None:
            kw["accum_out"] = accum.ap
            writes.append(accum)
        o, i = out.ap, in_.ap
        return self.op(eng, lambda e: e.activation(o, i, func, **kw), reads, writes)

    def tt(self, out, in0, in1, op, eng="dve"):
        o, a, b = out.ap, in0.ap, in1.ap
        return self.op(eng, lambda e: e.tensor_tensor(o, a, b, op), [in0, in1], [out])

    def ts(self, out, in0, s1, op0, s2=None, op1=None, eng="dve", accum=None):
        reads = [in0] + [s for s in (s1, s2) if isinstance(s, V)]
        o, a, x1, x2 = out.ap, in0.ap, _aps(s1), _aps(s2)
        writes = [out]
        kw = {}
        if accum is not None:
            kw["accum_out"] = accum.ap
            writes.append(accum)
        if op1 is None:
            return self.op(eng, lambda e: e.tensor_scalar(o, a, x1, None, op0, **kw), reads, writes)
        return self.op(eng, lambda e: e.tensor_scalar(o, a, x1, x2, op0, op1, **kw), reads, writes)

    def stt(self, out, in0, scalar, in1, op0, op1, eng="dve"):
        reads = [in0, in1] + ([scalar] if isinstance(scalar, V) else [])
        o, a, s, b = out.ap, in0.ap, _aps(scalar), in1.ap
        return self.op(eng, lambda e: e.scalar_tensor_tensor(o, a, s, b, op0, op1), reads, [out])

    def copy(self, out, in_, eng="dve"):
        o, i = out.ap, in_.ap
        if eng == "act":
            return self.op(eng, lambda e: e.copy(o, i), [in_], [out])
        return self.op(eng, lambda e: e.tensor_copy(o, i), [in_], [out])

    def memset(self, out, val, eng="dve"):
        o = out.ap
        return self.op(eng, lambda e: e.memset(o, val), [], [out])

    def scan(self, out, d0, d1, init, op0, op1, eng="dve"):
        o, a, b = out.ap, d0.ap, d1.ap
        ini = _aps(init)
        reads = [d0, d1] + ([init] if isinstance(init, V) else [])
        return self.op(eng, lambda e: e.tensor_tensor_scan(o, a, b, ini, op0, op1), reads, [out])

    def recip(self, out, in_):
        o, i = out.ap, in_.ap
        return self.op("dve", lambda e: e.reciprocal(o, i), [in_], [out])

    def reduce(self, out, in_, op, axis=AX.X, eng="dve"):
        o, i = out.ap, in_.ap
        return self.op(eng, lambda e: e.tensor_reduce(o, i, axis, op), [in_], [out])

    def emit(self):
        nc = self.nc
        for e in ENGS:
            for op in self.ops[e]:
                for d, raw in op.deps:
                    if _need(op, d, raw):
                        d.awaited = True
        esem = {e: self.es.enter_context(nc.semaphore(f"s_{e}")) for e in ENGS if e != "sp" or True}
        cnt = {e: 0 for e in ENGS}
        dma_toks = []
        for op in self.all_ops:
            e = op.eng
            if True:
                if op.isdma:
                    tk = op.dtok
                    if tk.dsem is None:
                        tk.dsem = self.es.enter_context(nc.semaphore(f"d_{tk.name}"))
                        dma_toks.append(tk)
                    tk.dcnt += 1
                    op.handle = (tk.dsem, op.incval * tk.dcnt)
                elif op.awaited:
                    cnt[e] += 1
                    op.handle = (esem[e], cnt[e])
        self.stats = dict(n_ops={e: len(self.ops[e]) for e in ENGS}, n_inc=dict(cnt), n_dsem=len(dma_toks))
        fw = {}
        for op in self.all_ops:
            if op.isdma and any(True for _ in [0]) and getattr(op, "outdram", False):
                sem, val = op.handle
                fw[id(sem)] = (sem, max(val, fw.get(id(sem), (sem, 0))[1]))
        final_waits = list(fw.values())

        def run(e, eng):
            known = {}
            for op in self.ops[e]:
                need = {}
                for d, raw in op.deps:
                    if not _need(op, d, raw):
                        continue
                    sem, val = d.handle
                    k = id(sem)
                    if known.get(k, 0) >= val:
                        continue
                    if k not in need or need[k][1] < val:
                        need[k] = (sem, val)
                for k, (sem, val) in need.items():
                    known[k] = val
                    eng.wait_ge(sem, val)
                ins = op.fn(eng)
                if ins is None:
                    continue
                if op.isdma or op.awaited:
                    sem, val = op.handle
                    ins.then_inc(sem, op.incval if op.isdma else 1)
            if e == "sp":
                for sem, val in final_waits:
                    eng.wait_ge(sem, val)

        with nc.Block() as block:
            @block.tensor
            def _(eng):
                run("pe", eng)

            @block.scalar
            def _(eng):
                run("act", eng)

            @block.vector
            def _(eng):
                run("dve", eng)

            @block.gpsimd
            def _(eng):
                run("pool", eng)

            @block.sync
            def _(eng):
                run("sp", eng)
        self.es.close()


NEG = -30000.0
DM = 1024
KT = 8
NF0 = 1280
F_GQ, F_GK, F_GV, F_GG, F_MQ, F_MK, F_MG = 0, 128, 256, 384, 448, 512, 576
F_RW = 640
F_XW, F_XA, F_XG, F_XV = 1024, 1088, 1152, 1280
NCOLP = 40


def host_consts():
    i = np.arange(128)
    r, c = i[:, None], i[None, :]
    f = np.float32
    parts = [np.eye(128, dtype=f),
             np.where(r < c, 0.0, NEG).astype(f),
             np.where(r <= c, 0.0, NEG).astype(f),
             np.where(c < r, 0.0, NEG).astype(f),
             (r < c).astype(f), (r <= c).astype(f), (c < r).astype(f)]
    return np.concatenate(parts, axis=1)


class Ctx:
    pass


def setup_common(P, consts_dram):
    C = Ctx()
    cst = P.sb([128, 7 * 128], name="cst")
    P.dma(cst[:], consts_dram[:])
    C.cst = cst
    C.ident = cst[:, 0:128]
    C.addT_s, C.addT_i, C.addD_s = cst[:, 128:256], cst[:, 256:384], cst[:, 384:512]
    C.mulT_s, C.mulT_i, C.mulD_s = cst[:, 512:640], cst[:, 640:768], cst[:, 768:896]
    C.ones = P.sb([128, 128], name="ones")
    P.memset(C.ones[:], 1.0)
    banks = [P.ps([128, 512], name=f"bank{i}") for i in range(8)]
    C.q = [Tsub(banks[i // 4].t[:, (i % 4) * 128:(i % 4 + 1) * 128], f"q{i}", banks[i // 4].tok) for i in range(16)]
    C.full = [banks[4], banks[5]]
    C.half = [Tsub(banks[6].t[:, 0:256], "h0", banks[6].tok), Tsub(banks[7].t[:, 0:256], "h1", banks[7].tok)]
    C.inv_ps = [C.q[2], C.q[6], C.q[10], C.q[14]]
    C.bankT = banks[0:4]
    C.banks = banks

    def reg(b, c0, nm):
        return Tsub(banks[b].t[:, c0:c0 + 128], nm, banks[b].tok)
    C.r = [reg(4, 256, "r0"), reg(4, 384, "r1"), reg(5, 0, "r2"), reg(5, 128, "r3"), reg(5, 256, "r4"), reg(5, 384, "r5"),
           reg(6, 256, "r6"), reg(6, 384, "r7"), reg(7, 256, "r8"), reg(7, 384, "r9")]
    C.inv_ps_r = [C.r[1], C.r[2], C.r[6], C.r[8]]
    return C


def tri_inverse(P, C, B, A, n, plus, X, XT, Pm, PTm, ps):
    idn = C.ident[0:n, 0:n]
    op = ALU.add if plus else ALU.subtract
    P.tt(X[0][0:n, 0:n], idn, B, op)
    P.tt(XT[0][0:n, 0:n], idn, A, op, eng="dve")
    nlev = int(np.log2(n)) - 1
    curP, curPT = B, A
    xi = 0
    for lev in range(nlev):
        last = lev == nlev - 1
        pi = lev % 2
        P.mm(ps[0][0:n, 0:n], curPT, curP)
        P.copy(Pm[pi][0:n, 0:n], ps[0][0:n, 0:n], eng="act")
        if not last:
            P.mm(ps[1][0:n, 0:n], curP, curPT)
            P.copy(PTm[pi][0:n, 0:n], ps[1][0:n, 0:n], eng="act")
        newP, newPT = Pm[pi][0:n, 0:n], PTm[pi][0:n, 0:n]
        P.mm(ps[2][0:n, 0:n], XT[xi][0:n, 0:n], newP)
        P.tt(X[1 - xi][0:n, 0:n], ps[2][0:n, 0:n], X[xi][0:n, 0:n], ALU.add)
        if not last:
            P.mm(ps[3][0:n, 0:n], newP, XT[xi][0:n, 0:n])
            P.tt(XT[1 - xi][0:n, 0:n], ps[3][0:n, 0:n], XT[xi][0:n, 0:n], ALU.add)
        xi = 1 - xi
        curP, curPT = newP, newPT
    return X[xi][0:n, 0:n]


def mixer_phase(P, C, T, SEG, dr, has_vres, pre="m", gath=None):
    NSEG = T // SEG
    NC128 = SEG // 128
    CR = 128
    NCR = SEG // CR
    NF = NF0 + (32 if has_vres else 0)
    sb = lambda shape, nm: P.sb(shape, name=f"{pre}s_{nm}")
    ident, ones = C.ident, C.ones
    q, half, full = C.q, C.half, C.full
    LP = MM_DT != F32
    wfm = P.sb([128, KT, NF], MM_DT, name=f"{pre}s_wfm")
    wtm = P.sb([128, KT, 384], MM_DT, name=f"{pre}s_wtm")
    wst = [sb([128, NF + 384], f"wst{i}") for i in range(2)] if LP else None
    for k in range(KT):
        if LP:
            w_ = wst[k % 2]
            P.dma(w_[:, 0:NF], dr["wfm"][k * 128:(k + 1) * 128, :])
            P.dma(w_[:, NF:NF + 384], dr["wtm"][k * 128:(k + 1) * 128, :])
            P.copy(wfm[:, k, :], w_[:, 0:NF], eng="dve")
            P.copy(wtm[:, k, :], w_[:, NF:NF + 384], eng="act")
        else:
            P.dma(wfm[:, k, :], dr["wfm"][k * 128:(k + 1) * 128, :])
            P.dma(wtm[:, k, :], dr["wtm"][k * 128:(k + 1) * 128, :])
    colp = sb([128, NCOLP], "colp")
    P.dma(colp[:], dr["colp"][:])
    rowp = sb([1, 4], "rowp")
    P.dma(rowp[:], dr["rowp"][:])
    bc = sb([128, 512], "bc")
    P.dma(bc[:], dr["bc"][:].pb(128))
    lora = sb([128, 512], "lora")
    P.dma(lora[:], dr["lora"][:])
    w2s, a2s, g2s, v2s = lora[0:64, 0:128], lora[0:64, 128:256], lora[:, 256:384], lora[0:32, 384:512]
    sc = sb([1, 8], "sc")
    P.act(sc[:, 0:1], rowp[:, 1:2], AF.Exp)
    P.ts(sc[:, 0:1], sc[:, 0:1], -1.0, ALU.mult)
    P.ts(sc[:, 1:3], rowp[:, 2:4], 1.0 / 15.0, ALU.mult)
    negA, dtb, bi15, bf15 = sc[:, 0:1], rowp[:, 0:1], sc[:, 1:2], sc[:, 2:3]
    P.ts(bc[:, 0:256], bc[:, 0:256], float(np.sqrt(128.0)), ALU.mult)
    for h in range(2):
        P.ts(bc[:, 256 + 128 * h:320 + 128 * h], bc[:, 256 + 128 * h:320 + 128 * h], 8.0, ALU.mult)
    ng_gdn, ng_ml = bc[:, 0:128], bc[:, 128:256]
    rm128 = sb([1, SEG], "rm128")
    P.memset(rm128[:], 1.0)
    P.memset(rm128[:].re("p (n c) -> p n c", c=128)[:, :, 0:1], 0.0)
    rmR = sb([64, SEG], "rmR")
    P.memset(rmR[:], 1.0)
    P.memset(rmR[:].re("p (n c) -> p n c", c=CR)[:, :, 0:1], 0.0)
    Sg = [sb([128, 128], f"Sg{i}") for i in range(2)]
    Sm = [sb([64, 129], f"Sm{i}") for i in range(2)]
    Sr = [[sb([64, 64], f"Sr{h}{i}") for i in range(2)] for h in range(2)]
    for t_ in (Sg[0], Sm[0], Sr[0][0], Sr[1][0]):
        P.memset(t_[:], 0.0)
    sgi, smi, sri = 0, 0, [0, 0]
    hTt = [sb([128, KT, SEG], f"hT{i}") for i in range(2)]
    hTb = [P.sb([128, KT, SEG], MM_DT, name=f"{pre}s_hTb{i}") for i in range(2)] if LP else hTt
    xq, xk, xv = (sb([128, SEG + 3], n_) for n_ in ("xq", "xk", "xv"))
    for t_ in (xq, xk, xv):
        P.memset(t_[:, SEG:SEG + 3], 0.0)
    gq, gk, gv, gsq = (sb([128, SEG], n_) for n_ in ("gq", "gk", "gv", "gsq"))
    gacc = sb([128, SEG], "gacc")
    gg = sb([64, SEG], "gg")
    mq, mk, mg = (sb([64, SEG], n_) for n_ in ("mq", "mk", "mg"))
    zr = [sb([64, SEG + 1], f"zr{h}") for h in range(2)]
    zk = [sb([64, SEG + 1], f"zk{h}") for h in range(2)]
    zv = [sb([64, SEG + 1], f"zv{h}") for h in range(2)]
    zxw, zxa = sb([64, SEG + 1], "zxw"), sb([64, SEG + 1], "zxa")
    zxg = sb([128, SEG + 1], "zxg")
    zxv = sb([32, SEG + 1], "zxv") if has_vres else None
    halo1 = zr + zk + zv + [zxw, zxa, zxg] + ([zxv] if has_vres else [])
    for t_ in halo1:
        P.memset(t_[:, SEG:SEG + 1], 0.0)
    gz = sb([128, NC128, 128], "gz")
    mva = sb([128, NC128, 129], "mva")
    P.memset(mva[:, :, 128:129], 1.0)
    mo = sb([128, NC128, 128], "mo")
    nrow = 16
    rows = [sb([1, SEG], f"row{i}") for i in range(nrow)]
    yst = sb([128, SEG], "yst")
    ystm = sb([128, SEG], "ystm")
    ystr = sb([64, SEG], "ystr")
    def set2(shape, nm):
        return [sb(shape, f"{nm}{i}") for i in range(2)]
    cols, MAT, MA, MQT, Bm, Am, QKm = (set2([128, 128], n_) for n_ in ("cols", "MAT", "MA", "MQT", "Bm", "Am", "QKm"))
    kbg, kd, vb, negw, vnew, osb, ysb, ngz = (set2([128, 129], n_) for n_ in ("kbg", "kd", "vb", "negw", "vnew", "osb", "ysb", "ngz"))
    ssq = set2([128, 4], "ssq")
    X, XT, Pm, PTm = (set2([128, 128], n_) for n_ in ("X", "XT", "Pm", "PTm"))
    X_r, XT_r, Pm_r, PTm_r = (set2([128, 128], n_) for n_ in ("Xr", "XTr", "Pmr", "PTmr"))
    cols_r, Am_r = set2([128, 128], "colsr"), set2([128, 128], "Amr")
    osb_r, ysb_r, vnew_r = (set2([128, 129], n_) for n_ in ("osbr", "ysbr", "vnewr"))
    ssq_r = set2([128, 4], "ssqr")
    rr, rk, rvv = sb([64, SEG], "rr"), sb([64, SEG], "rk"), sb([64, SEG], "rvv")
    xw, xa = sb([64, SEG], "xw"), sb([64, SEG], "xa")
    xg = sb([128, SEG], "xg")
    xvt = sb([32, SEG], "xvt") if has_vres else None
    tmpd = sb([128, SEG], "tmpd")
    lw, logw, av, kkv, k2v, E1, E2, E3, tend, behat, khat, beend, kend, prod = (
        sb([64, SEG], n_) for n_ in ("lw", "logw", "av", "kkv", "k2v", "E1", "E2", "E3", "tend", "behat", "khat", "beend", "kend", "prod"))
    AR = sb([64, NCR, 2, CR], "AR")
    vfs = sb([64, SEG], "vfs") if has_vres else None
    GM, TK = set2([128, 512], "GM"), set2([128, 256], "TK")
    wu, lakv, usb = set2([64, 128], "wu"), set2([128, 64], "lakv"), set2([128, 64], "usb")
    hT_d = dr["hT"]

    for sg in range(NSEG):
        t0 = sg * SEG
        hb = hTt[sg % 2]
        if gath is None:
            P.dma(hb[:], hT_d[:, t0:t0 + SEG].re("(k p) t -> p k t", p=128))
        else:
            rk_, c0_ = t0 // gath, t0 % gath
            for k in range(KT):
                P.dma(hb[:, k, :], hT_d[k][rk_][:, c0_:c0_ + SEG])
        if LP:
            P.copy(hTb[sg % 2][:, 0:4, :], hb[:, 0:4, :], eng="dve")
            P.copy(hTb[sg % 2][:, 4:8, :], hb[:, 4:8, :], eng="act")
            hb = hTb[sg % 2]
        for t_ in (xq, xk, xv):
            P.copy(t_[:, 0:3], t_[:, SEG:SEG + 3], eng="dve")
        for t_ in halo1:
            P.copy(t_[:, 0:1], t_[:, SEG:SEG + 1], eng="dve")
        fi = [0]

        def proj_fm(c0, nr, evac):
            bank = full[fi[0] % 2]
            fi[0] += 1
            for k in range(KT):
                P.mm(bank[0:nr, 0:SEG], wfm[:, k, c0:c0 + nr], hb[:, k, :], start=(k == 0), stop=(k == KT - 1))
            evac(bank[0:nr, 0:SEG])

        proj_fm(F_GQ, 128, lambda p_: P.copy(xq[:, 3:], p_, eng="act"))
        proj_fm(F_GK, 128, lambda p_: P.copy(xk[:, 3:], p_, eng="dve"))
        proj_fm(F_GV, 128, lambda p_: P.copy(xv[:, 3:], p_, eng="act"))
        proj_fm(F_GG, 64, lambda p_: P.copy(gg[:], p_, eng="dve"))
        proj_fm(F_MQ, 64, lambda p_: P.copy(mq[:], p_, eng="act"))
        proj_fm(F_MK, 64, lambda p_: P.copy(mk[:], p_, eng="dve"))
        proj_fm(F_MG, 64, lambda p_: P.copy(mg[:], p_, eng="act"))
        for h in range(2):
            proj_fm(F_RW + h * 192, 64, lambda p_, h=h: P.copy(zr[h][:, 1:], p_, eng="dve"))
            proj_fm(F_RW + h * 192 + 64, 64, lambda p_, h=h: P.copy(zk[h][:, 1:], p_, eng="act"))
            proj_fm(F_RW + h * 192 + 128, 64, lambda p_, h=h: P.copy(zv[h][:, 1:], p_, eng="dve"))
        proj_fm(F_XW, 64, lambda p_: P.copy(zxw[:, 1:], p_, eng="act"))
        proj_fm(F_XA, 64, lambda p_: P.copy(zxa[:, 1:], p_, eng="dve"))
        proj_fm(F_XG, 128, lambda p_: P.copy(zxg[:, 1:], p_, eng="act"))
        if has_vres:
            proj_fm(F_XV, 32, lambda p_: P.copy(zxv[:, 1:], p_, eng="dve"))
        for j in range(NC128):
            bank = full[fi[0] % 2]
            fi[0] += 1
            for k in range(KT):
                P.mm(bank[:, 0:384], hb[:, k, j * 128:(j + 1) * 128], wtm[:, k, :], start=(k == 0), stop=(k == KT - 1))
            P.act(gz[:, j, :], bank[:, 0:128], AF.Silu)
            P.copy(mva[:, j, 0:128], bank[:, 128:256], eng="dve")
            P.act(mo[:, j, :], bank[:, 256:384], AF.Sigmoid)

        P.begin_defer()
        for src, dst, cb in ((xq, gq, 0), (xk, gk, 4), (xv, gv, 8)):
            P.ts(gacc[:], src[:, 0:SEG], colp[:, cb:cb + 1], ALU.mult)
            for j in range(1, 4):
                P.stt(gacc[:], src[:, j:j + SEG], colp[:, cb + j:cb + j + 1], gacc[:], ALU.mult, ALU.add)
            P.act(dst[:], gacc[:], AF.Silu)
        (r_gs, r_gc, r_lb, r_lrk, r_lrq, r_A1, r_B1, r_Q, r_ekbg, r_ekd, r_ebeta, r_eq, r_egl, r_t0, r_t1, r_t2) = rows
        P.act(r_t0[:], gg[0:1, :], AF.Exp, bias=dtb)
        P.act(r_t0[:], r_t0[:], AF.Ln, bias=1.0)
        P.ts(r_gs[:], r_t0[:], negA, ALU.mult)
        P.scan(r_gc[:], rm128[:], r_gs[:], 0.0, ALU.mult, ALU.add)
        P.act(r_t1[:], gg[32:33, :], AF.Exp, scale=-1.0)
        P.act(r_t1[:], r_t1[:], AF.Ln, bias=1.0)
        P.ts(r_lb[:], r_t1[:], -1.0, ALU.mult)
        for src, dst, addc in ((gk, r_lrk, 0.0), (gq, r_lrq, float(np.log(128.0 ** -0.5)))):
            P.act(gsq[:], src[:], AF.Square)
            P.mm(C.bankT[3][0:1, 0:SEG], ones[:, 0:1], gsq[:])
            P.act(r_t2[:], C.bankT[3][0:1, 0:SEG], AF.Ln, bias=1e-6)
            P.ts(dst[:], r_t2[:], -0.5, ALU.mult, addc, ALU.add)
        P.tt(r_A1[:], r_lrk[:], r_lb[:], ALU.add)
        P.tt(r_A1[:], r_A1[:], r_gc[:], ALU.add)
        P.tt(r_B1[:], r_lrk[:], r_gc[:], ALU.subtract)
        P.tt(r_Q[:], r_lrq[:], r_gc[:], ALU.add)
        P.act(r_ekbg[:], r_A1[:], AF.Exp)
        P.act(r_eq[:], r_Q[:], AF.Exp)
        P.act(r_ebeta[:], r_lb[:], AF.Exp)
        glast = r_gc[:].re("p (n c) -> p n c", c=128)[:, :, 127:128]
        P.tt(r_t0[:].re("p (n c) -> p n c", c=128), r_B1[:].re("p (n c) -> p n c", c=128), glast.bt([1, NC128, 128]), ALU.add)
        P.act(r_ekd[:], r_t0[:], AF.Exp)
        P.act(r_egl[:, 0:NC128], r_gc[:].re("p (n c) -> p n c", c=128)[:, :, 127], AF.Exp)
        for j in range(NC128):
            ci = j % 2
            cs = slice(j * 128, (j + 1) * 128)
            S_old, S_new = Sg[sgi], Sg[1 - sgi]
            sgi = 1 - sgi
            for x_, r_ in enumerate((r_ekbg, r_ekd, r_ebeta, r_eq)):
                P.mm(q[0][:, x_:x_ + 1], r_[:, cs], ones[0:1, 0:1])
            P.mm(q[0][:, 4:5], ones[0:1, :], r_egl[:, j:j + 1])
            P.copy(cols[ci][:, 0:5], q[0][:, 0:5], eng="act")
            c_kbg, c_kd, c_beta, c_eq, c_gl = (cols[ci][:, x_:x_ + 1] for x_ in range(5))
            for ps_, lrow, rrow, msk, dst in ((q[1], r_B1, r_A1, C.addT_s, MAT[ci]), (q[2], r_A1, r_B1, C.addD_s, MA[ci]),
                                              (q[3], r_B1, r_Q, C.addT_i, MQT[ci])):
                P.mm(ps_[:, :], lrow[:, cs], ones[0:1, :], start=True, stop=False)
                P.mm(ps_[:, :], ones[0:1, :], rrow[:, cs], start=False, stop=False)
                P.mm(ps_[:, :], ident, msk, start=False, stop=True)
                P.act(dst[:], ps_[:, :], AF.Exp)
            P.mm(q[4][:, :], gk[:, cs], gk[:, cs])
            P.mm(q[5][:, :], gk[:, cs], gq[:, cs])
            P.tt(Bm[ci][:], q[4][:, :], MAT[ci][:], ALU.mult)
            P.tt(Am[ci][:], q[4][:, :], MA[ci][:], ALU.mult)
            P.tt(QKm[ci][:], q[5][:, :], MQT[ci][:], ALU.mult)
            P.mm(q[6][:, :], gk[:, cs], ident)
            P.mm(q[7][:, :], gv[:, cs], ident)
            P.act(kbg[ci][:, 0:128], q[6][:, :], AF.Identity, scale=c_kbg)
            P.ts(kd[ci][:, 0:128], q[6][:, :], c_kd, ALU.mult)
            P.act(vb[ci][:, 0:128], q[7][:, :], AF.Identity, scale=c_beta)
            R = tri_inverse(P, C, Bm[ci][:], Am[ci][:], 128, False, X, XT, Pm, PTm, C.inv_ps)
            P.mm(q[13][:, :], kbg[ci][:, 0:128], R)
            P.act(negw[ci][:, 0:128], q[13][:, :], AF.Identity, scale=-1.0)
            P.mm(q[12][:, :], R, vb[ci][:, 0:128], start=True, stop=False)
            P.mm(q[12][:, :], negw[ci][:, 0:128], S_old[:], start=False, stop=True)
            P.copy(vnew[ci][:, 0:128], q[12][:, :], eng="act")
            P.mm(q[14][:, :], gq[:, cs], S_old[:])
            P.mm(q[15][:, :], QKm[ci][:], vnew[ci][:, 0:128])
            P.mm(q[1][:, :], kd[ci][:, 0:128], vnew[ci][:, 0:128])
            P.stt(S_new[:], S_old[:], c_gl, q[1][:, :], ALU.mult, ALU.add)
            P.act(osb[ci][:, 0:128], q[14][:, :], AF.Identity, scale=c_eq)
            P.tt(osb[ci][:, 0:128], osb[ci][:, 0:128], q[15][:, :], ALU.add)
            P.act(ysb[ci][:, 0:128], osb[ci][:, 0:128], AF.Square, accum=ssq[ci][:, 0:1])
            P.act(ssq[ci][:, 1:2], ssq[ci][:, 0:1], AF.Ln, bias=128.0 * 1e-6)
            P.act(ssq[ci][:, 1:2], ssq[ci][:, 1:2], AF.Exp, scale=-0.5)
            P.tt(ngz[ci][:, 0:128], gz[:, j, :], ng_gdn, ALU.mult, eng="dve")
            P.stt(ysb[ci][:, 0:128], osb[ci][:, 0:128], ssq[ci][:, 1:2], ngz[ci][:, 0:128], ALU.mult, ALU.mult)
            P.mm(q[2][:, :], ysb[ci][:, 0:128], ident)
            P.copy(yst[:, cs], q[2][:, :], eng="act")
        P.dma(dr["yT"][0:128, t0:t0 + SEG], yst[:])

        (r_li, r_lf, r_F, r_Ds, r_ee, r_ekw, r_egl2, r_u0, r_u1) = rows[0:9]
        P.act(r_u0[:], mg[0:1, :], AF.Tanh, scale=1.0 / 15.0, bias=bi15)
        P.ts(r_li[:], r_u0[:], 15.0, ALU.mult)
        P.act(r_u1[:], mg[32:33, :], AF.Tanh, scale=1.0 / 15.0, bias=bf15)
        P.act(r_u1[:], r_u1[:], AF.Exp, scale=-15.0)
        P.act(r_u1[:], r_u1[:], AF.Ln, bias=1.0)
        P.ts(r_lf[:], r_u1[:], -1.0, ALU.mult)
        P.scan(r_F[:], rm128[:], r_lf[:], 0.0, ALU.mult, ALU.add)
        lsc = float(np.log(64.0 ** -0.5))
        P.stt(r_Ds[:], r_li[:], lsc, r_F[:], ALU.add, ALU.subtract)
        P.act(r_ee[:], r_F[:], AF.Exp)
        Flast = r_F[:].re("p (n c) -> p n c", c=128)[:, :, 127:128]
        P.tt(r_u0[:].re("p (n c) -> p n c", c=128), r_Ds[:].re("p (n c) -> p n c", c=128), Flast.bt([1, NC128, 128]), ALU.add)
        P.act(r_ekw[:], r_u0[:], AF.Exp)
        P.act(r_egl2[:, 0:NC128], r_F[:].re("p (n c) -> p n c", c=128)[:, :, 127], AF.Exp)
        for j in range(NC128):
            ci = j % 2
            cs = slice(j * 128, (j + 1) * 128)
            S_old, S_new = Sm[smi], Sm[1 - smi]
            smi = 1 - smi
            P.mm(q[0][:, 0:1], r_ee[:, cs], ones[0:1, 0:1])
            P.mm(q[0][:, 1:2], r_ekw[:, cs], ones[0:1, 0:1])
            P.mm(q[0][:, 2:3], ones[0:1, :], r_egl2[:, j:j + 1])
            P.copy(cols[ci][:, 0:3], q[0][:, 0:3], eng="act")
            c_e, c_kw, c_gl = (cols[ci][:, x_:x_ + 1] for x_ in range(3))
            P.mm(q[1][:, :], r_Ds[:, cs], ones[0:1, :], start=True, stop=False)
            P.mm(q[1][:, :], ones[0:1, :], r_F[:, cs], start=False, stop=False)
            P.mm(q[1][:, :], ident, C.addT_i, start=False, stop=True)
            P.act(MAT[ci][:], q[1][:, :], AF.Exp)
            P.mm(q[4][:, :], mk[:, cs], mq[:, cs])
            P.tt(Bm[ci][:], q[4][:, :], MAT[ci][:], ALU.mult)
            P.mm(q[6][:, 0:64], mk[:, cs], ident[0:64, 0:64])
            P.ts(kd[ci][:, 0:64], q[6][:, 0:64], c_kw, ALU.mult)
            P.mm(C.bankT[2][:, 0:129], Bm[ci][:], mva[:, j, :])
            P.mm(C.bankT[3][:, 0:129], mq[:, cs], S_old[:])
            P.mm(q[1][0:64, :], kd[ci][:, 0:64], mva[:, j, 0:128])
            P.mm(q[2][0:64, 0:1], kd[ci][:, 0:64], mva[:, j, 128:129])
            P.stt(S_new[:, 0:128], S_old[:, 0:128], c_gl[0:64, :], q[1][0:64, :], ALU.mult, ALU.add)
            P.stt(S_new[:, 128:129], S_old[:, 128:129], c_gl[0:64, :], q[2][0:64, 0:1], ALU.mult, ALU.add)
            P.act(osb[ci][:, 0:129], C.bankT[3][:, 0:129], AF.Identity, scale=c_e)
            P.tt(osb[ci][:, 0:129], osb[ci][:, 0:129], C.bankT[2][:, 0:129], ALU.add)
            P.act(ssq[ci][:, 0:1], osb[ci][:, 128:129], AF.Abs)
            P.ts(ssq[ci][:, 0:1], ssq[ci][:, 0:1], 1.0, ALU.max)
            P.recip(ssq[ci][:, 1:2], ssq[ci][:, 0:1])
            P.act(vnew[ci][:, 0:128], osb[ci][:, 0:128], AF.Identity, scale=ssq[ci][:, 1:2])
            P.act(ysb[ci][:, 0:128], vnew[ci][:, 0:128], AF.Square, accum=ssq[ci][:, 2:3])
            P.act(ssq[ci][:, 3:4], ssq[ci][:, 2:3], AF.Ln, bias=128.0 * 1e-6)
            P.act(ssq[ci][:, 3:4], ssq[ci][:, 3:4], AF.Exp, scale=-0.5)
            P.tt(ngz[ci][:, 0:128], mo[:, j, :], ng_ml, ALU.mult, eng="dve")
            P.stt(ysb[ci][:, 0:128], vnew[ci][:, 0:128], ssq[ci][:, 3:4], ngz[ci][:, 0:128], ALU.mult, ALU.mult)
            P.mm(q[3][:, :], ysb[ci][:, 0:128], ident)
            P.copy(ystm[:, cs], q[3][:, :], eng="act")
        P.dma(dr["yT"][128:256, t0:t0 + SEG], ystm[:])

        LA = P.end_defer()
        P.begin_defer()
        def shift(dst, z, mucol, npart):
            P.tt(tmpd[0:npart, :], z[0:npart, 0:SEG], z[0:npart, 1:SEG + 1], ALU.subtract)
            P.stt(dst[0:npart, :], tmpd[0:npart, :], mucol, z[0:npart, 1:SEG + 1], ALU.mult, ALU.add)
        shift(xw, zxw, colp[0:64, 36:37], 64)
        shift(xa, zxa, colp[0:64, 37:38], 64)
        shift(xg, zxg, colp[:, 38:39], 128)
        if has_vres:
            shift(xvt, zxv, colp[0:32, 39:40], 32)
        P.act(xw[:], xw[:], AF.Tanh)
        P.act(xg[:], xg[:], AF.Sigmoid)
        for h in range(2):
            cb = 12 + h * 12
            cp = lambda x_: colp[0:64, cb + x_:cb + x_ + 1]
            hs = slice(h * 64, (h + 1) * 64)
            shift(rr, zr[h], cp(0), 64)
            shift(rk, zk[h], cp(1), 64)
            shift(rvv, zv[h], cp(2), 64)
            P.mm(full[0][0:64, 0:SEG], w2s[:, hs], xw[:])
            P.act(logw[:], full[0][0:64, 0:SEG], AF.Sigmoid, bias=cp(3))
            P.ts(logw[:], logw[:], -float(np.exp(-0.5)), ALU.mult)
            P.mm(full[1][0:64, 0:SEG], a2s[:, hs], xa[:])
            P.act(av[:], full[1][0:64, 0:SEG], AF.Sigmoid, bias=cp(4))
            if has_vres:
                P.dma(vfs[:], dr["vfirst_in"][h * 64:(h + 1) * 64, t0:t0 + SEG])
                P.mm(full[0][0:64, 0:SEG], v2s[:, hs], xvt[:])
                P.act(tmpd[0:64, :], full[0][0:64, 0:SEG], AF.Sigmoid, bias=cp(8))
                P.tt(vfs[:], vfs[:], rvv[:], ALU.subtract)
                P.tt(vfs[:], vfs[:], tmpd[0:64, :], ALU.mult)
                P.tt(rvv[:], rvv[:], vfs[:], ALU.add)
            else:
                P.dma(dr["vfirst_out"][h * 64:(h + 1) * 64, t0:t0 + SEG], rvv[:])
            P.ts(kkv[:], rk[:], cp(5), ALU.mult)
            P.act(tmpd[0:64, :], kkv[:], AF.Square)
            P.mm(full[0][0:64, 0:SEG], ones[0:64, 0:64], tmpd[0:64, :])
            P.act(tmpd[0:64, :], full[0][0:64, 0:SEG], AF.Ln, bias=1e-6)
            P.act(tmpd[0:64, :], tmpd[0:64, :], AF.Exp, scale=-0.5)
            P.tt(kkv[:], kkv[:], tmpd[0:64, :], ALU.mult)
            P.ts(tmpd[0:64, :], av[:], -1.0, ALU.add, cp(6), ALU.mult)
            P.stt(k2v[:], tmpd[0:64, :], 1.0, rk[:], ALU.add, ALU.mult)
            P.stt(prod[:], rr[:], cp(7), k2v[:], ALU.mult, ALU.mult)
            P.scan(lw[:], rmR[:], logw[:], 0.0, ALU.mult, ALU.add)
            P.act(E1[:], lw[:], AF.Exp)
            P.act(E2[:], lw[:], AF.Exp, scale=-1.0)
            P.tt(tmpd[0:64, :], lw[:], logw[:], ALU.subtract)
            P.act(E3[:], tmpd[0:64, :], AF.Exp)
            cview = lambda t_: t_.re("p (n c) -> p n c", c=CR)
            lwl = cview(lw[:])[:, :, CR - 1:CR]
            P.tt(cview(tmpd[0:64, :]), cview(lw[:]), lwl.bt([64, NCR, CR]), ALU.subtract)
            P.act(tend[:], tmpd[0:64, :], AF.Exp, scale=-1.0)
            ARv = AR[:]
            P.stt(ARv[:, :, 0, :], cview(kkv[:]), -1.0, cview(E3[:]), ALU.mult, ALU.mult)
            P.tt(ARv[:, :, 1, :], cview(rr[:]), cview(E1[:]), ALU.mult)
            P.tt(tmpd[0:64, :], kkv[:], av[:], ALU.mult)
            P.tt(behat[:], tmpd[0:64, :], E2[:], ALU.mult)
            P.tt(beend[:], tmpd[0:64, :], tend[:], ALU.mult)
            P.tt(khat[:], k2v[:], E2[:], ALU.mult)
            P.tt(kend[:], k2v[:], tend[:], ALU.mult)
            lg_b = bc[0:CR, 256 + 128 * h:320 + 128 * h]
            lb_b = bc[0:CR, 320 + 128 * h:384 + 128 * h]
            for j in range(NCR):
                ci = j % 2
                cs = slice(j * CR, (j + 1) * CR)
                S_old, S_new = Sr[h][sri[h]], Sr[h][1 - sri[h]]
                sri[h] = 1 - sri[h]
                albar = ARv[:, j, 0, :]
                rbar = ARv[:, j, 1, :]
                arv = ARv[:, j, :, :]
                P.mm(half[0][0:CR, 0:2 * CR], behat[:, cs], arv)
                P.mm(half[1][0:CR, 0:2 * CR], khat[:, cs], arv)
                P.mm(C.r[0][0:CR, 0:CR], albar, behat[:, cs])
                gm = GM[ci]
                mS, mI = C.mulT_s[0:CR, 0:CR], C.mulT_i[0:CR, 0:CR]
                P.tt(gm[0:CR, 0:CR], half[0][0:CR, 0:CR], mS, ALU.mult)
                P.tt(gm[0:CR, CR:2 * CR], half[0][0:CR, CR:2 * CR], mI, ALU.mult)
                P.tt(gm[0:CR, 2 * CR:3 * CR], half[1][0:CR, 0:CR], mS, ALU.mult)
                P.tt(gm[0:CR, 3 * CR:4 * CR], half[1][0:CR, CR:2 * CR], mI, ALU.mult)
                P.tt(Am_r[ci][0:CR, 0:CR], C.r[0][0:CR, 0:CR], C.mulD_s[0:CR, 0:CR], ALU.mult)
                for x_, src in enumerate((rvv[:, cs], albar, beend[:, cs], kend[:, cs])):
                    P.mm(full[0][0:CR, x_ * 64:(x_ + 1) * 64], src, ident[0:64, 0:64])
                tk = TK[ci]
                P.copy(tk[0:CR, :], full[0][0:CR, 0:256], eng="act")
                v_t, al_t, be_t, ke_t = (tk[0:CR, x_ * 64:(x_ + 1) * 64] for x_ in range(4))
                R = tri_inverse(P, C, gm[0:CR, 0:CR], Am_r[ci][0:CR, 0:CR], CR, True, X_r, XT_r, Pm_r, PTm_r, C.inv_ps_r)
                P.mm(C.r[3][0:64, 0:CR], al_t, R)
                P.copy(wu[ci][:, 0:CR], C.r[3][0:64, 0:CR], eng="act")
                P.mm(C.r[4][0:CR, 0:64], gm[0:CR, 2 * CR:3 * CR], v_t)
                P.copy(lakv[ci][0:CR, :], C.r[4][0:CR, 0:64], eng="dve")
                P.mm(C.r[5][0:CR, 0:64], R, lakv[ci][0:CR, :], start=True, stop=False)
                P.mm(C.r[5][0:CR, 0:64], wu[ci][:, 0:CR], S_old[:], start=False, stop=True)
                P.copy(usb[ci][0:CR, :], C.r[5][0:CR, 0:64], eng="act")
                P.mm(C.r[7][0:CR, 0:64], rbar, S_old[:], start=True, stop=False)
                P.mm(C.r[7][0:CR, 0:64], gm[0:CR, CR:2 * CR], usb[ci][0:CR, :], start=False, stop=False)
                P.mm(C.r[7][0:CR, 0:64], gm[0:CR, 3 * CR:4 * CR], v_t, start=False, stop=True)
                P.mm(C.r[9][0:64, 0:64], be_t, usb[ci][0:CR, :], start=True, stop=False)
                P.mm(C.r[9][0:64, 0:64], ke_t, v_t, start=False, stop=True)
                P.stt(S_new[:], S_old[:], E1[:, j * CR + CR - 1:j * CR + CR], C.r[9][0:64, 0:64], ALU.mult, ALU.add)
                ob = osb_r[ci]
                P.copy(ob[0:CR, 0:64], C.r[7][0:CR, 0:64], eng="act")
                sq_ = ssq_r[ci]
                P.reduce(sq_[0:CR, 0:1], ob[0:CR, 0:64], ALU.add)
                P.ts(sq_[0:CR, 1:2], sq_[0:CR, 0:1], -1.0 / 64.0, ALU.mult)
                P.act(ob[0:CR, 64:128], ob[0:CR, 0:64], AF.Identity, bias=sq_[0:CR, 1:2])
                P.act(ysb_r[ci][0:CR, 0:64], ob[0:CR, 64:128], AF.Square, accum=sq_[0:CR, 2:3])
                P.act(sq_[0:CR, 3:4], sq_[0:CR, 2:3], AF.Ln, bias=64.0 * 64e-5)
                P.act(sq_[0:CR, 3:4], sq_[0:CR, 3:4], AF.Exp, scale=-0.5)
                P.stt(ysb_r[ci][0:CR, 0:64], ob[0:CR, 64:128], sq_[0:CR, 3:4], lg_b, ALU.mult, ALU.mult)
                P.tt(ysb_r[ci][0:CR, 0:64], ysb_r[ci][0:CR, 0:64], lb_b, ALU.add)
                P.mm(C.r[0][0:CR, 0:1], prod[:, cs], ones[0:64, 0:1])
                P.copy(cols_r[ci][0:CR, 0:1], C.r[0][0:CR, 0:1], eng="act")
                P.stt(ysb_r[ci][0:CR, 64:128], v_t, cols_r[ci][0:CR, 0:1], ysb_r[ci][0:CR, 0:64], ALU.mult, ALU.add)
                P.mm(C.r[3][0:CR, 0:64], xg[:, cs], g2s[:, hs])
                P.tt(vnew_r[ci][0:CR, 0:64], ysb_r[ci][0:CR, 64:128], C.r[3][0:CR, 0:64], ALU.mult)
                P.mm(C.r[4][0:64, 0:CR], vnew_r[ci][0:CR, 0:64], ident[0:CR, 0:CR])
                P.copy(ystr[:, cs], C.r[4][0:64, 0:CR], eng="act")
            P.dma(dr["yT"][256 + 64 * h:320 + 64 * h, t0:t0 + SEG], ystr[:])
        LB = P.end_defer()
        P.merge([LA, LB])


GDN_OFF, ML_OFF, RW_OFF, GATE_OFF = 0, 2056, 3600, 5392


def prep_mixer_inputs(inp, l, hq):
    f = np.float32
    has_vres = l > 0
    w_in = inp["w_in"][l]
    NF = NF0 + (32 if has_vres else 0)
    wfm = np.zeros((DM, NF), f)
    g0 = GDN_OFF
    wfm[:, F_GQ:F_GQ + 128] = w_in[:, g0 + hq * 128:g0 + (hq + 1) * 128]
    wfm[:, F_GK:F_GK + 128] = w_in[:, g0 + 512 + hq * 128:g0 + 512 + (hq + 1) * 128]
    wfm[:, F_GV:F_GV + 128] = w_in[:, g0 + 1024 + hq * 128:g0 + 1024 + (hq + 1) * 128]
    wfm[:, F_GG] = w_in[:, g0 + 2048 + hq]
    wfm[:, F_GG + 32] = w_in[:, g0 + 2052 + hq]
    m0 = ML_OFF
    wfm[:, F_MQ:F_MQ + 64] = w_in[:, m0 + hq * 64:m0 + (hq + 1) * 64]
    wfm[:, F_MK:F_MK + 64] = w_in[:, m0 + 256 + hq * 64:m0 + 256 + (hq + 1) * 64]
    wfm[:, F_MG] = w_in[:, m0 + 1536 + hq]
    wfm[:, F_MG + 32] = w_in[:, m0 + 1540 + hq]
    r0 = RW_OFF
    for h in range(2):
        hh = 2 * hq + h
        for x_ in range(3):
            wfm[:, F_RW + h * 192 + x_ * 64:F_RW + h * 192 + (x_ + 1) * 64] = w_in[:, r0 + x_ * 512 + hh * 64:r0 + x_ * 512 + (hh + 1) * 64]
    wfm[:, F_XW:F_XW + 64] = w_in[:, r0 + 1536:r0 + 1600]
    wfm[:, F_XA:F_XA + 64] = w_in[:, r0 + 1600:r0 + 1664]
    wfm[:, F_XG:F_XG + 128] = w_in[:, r0 + 1664:r0 + 1792]
    if has_vres:
        wfm[:, F_XV:F_XV + 32] = inp["rwkv_v1"][l - 1]
    wtm = np.zeros((DM, 384), f)
    wtm[:, 0:128] = w_in[:, g0 + 1536 + hq * 128:g0 + 1536 + (hq + 1) * 128]
    wtm[:, 128:256] = w_in[:, m0 + 512 + hq * 128:m0 + 512 + (hq + 1) * 128]
    wtm[:, 256:384] = w_in[:, m0 + 1024 + hq * 128:m0 + 1024 + (hq + 1) * 128]
    colp = np.zeros((128, NCOLP), f)
    conv = inp["gdn_conv"][l]
    for x_ in range(3):
        colp[:, x_ * 4:(x_ + 1) * 4] = conv[:, x_ * 512 + hq * 128:x_ * 512 + (hq + 1) * 128].T
    mu = inp["rwkv_mu"][l]
    for h in range(2):
        hh = 2 * hq + h
        cb = 12 + h * 12
        sl = slice(hh * 64, (hh + 1) * 64)
        colp[0:64, cb + 0] = mu[0:512][sl]
        colp[0:64, cb + 1] = mu[512:1024][sl]
        colp[0:64, cb + 2] = mu[1024:1536][sl]
        colp[0:64, cb + 3] = inp["rwkv_w0"][l][sl]
        colp[0:64, cb + 4] = inp["rwkv_a0"][l][sl]
        colp[0:64, cb + 5] = inp["rwkv_k_k"][l][sl]
        colp[0:64, cb + 6] = inp["rwkv_k_a"][l][sl]
        colp[0:64, cb + 7] = inp["rwkv_r_k"][l].reshape(-1)[sl]
        if has_vres:
            colp[0:64, cb + 8] = inp["rwkv_v0"][l - 1][sl]
    colp[0:64, 36] = mu[1536:1600]
    colp[0:64, 37] = mu[1600:1664]
    colp[:, 38] = mu[1664:1792]
    if has_vres:
        colp[0:32, 39] = inp["rwkv_mu_v1"][l - 1]
    rowp = np.array([[inp["gdn_dt_bias"][l][hq], inp["gdn_a_log"][l][hq], inp["mlstm_b_i"][l][hq], inp["mlstm_b_f"][l][hq]]], f)
    bc = np.zeros((1, 512), f)
    bc[0, 0:128] = inp["gdn_norm_g"][l]
    bc[0, 128:256] = inp["mlstm_norm_g"][l][hq * 128:(hq + 1) * 128]
    for h in range(2):
        hh = 2 * hq + h
        bc[0, 256 + 128 * h:320 + 128 * h] = inp["rwkv_lnx_g"][l][hh * 64:(hh + 1) * 64]
        bc[0, 320 + 128 * h:384 + 128 * h] = inp["rwkv_lnx_b"][l][hh * 64:(hh + 1) * 64]
    lora = np.zeros((128, 512), f)
    cs = slice(hq * 128, (hq + 1) * 128)
    lora[0:64, 0:128] = inp["rwkv_w2"][l][:, cs]
    lora[0:64, 128:256] = inp["rwkv_a2"][l][:, cs]
    lora[:, 256:384] = inp["rwkv_g2"][l][:, cs]
    if has_vres:
        lora[0:32, 384:512] = inp["rwkv_v2"][l - 1][:, cs]
    return dict(wfm=wfm, wtm=wtm, colp=colp, rowp=rowp, bc=bc, lora=lora)


def declare_mixer_dram(P, T, has_vres, pre, ext=True):
    NF = NF0 + (32 if has_vres else 0)
    kin = "ExternalInput" if ext else "Internal"
    dr = {}
    for nm, shp in (("wfm", [DM, NF]), ("wtm", [DM, 384]), ("colp", [128, NCOLP]), ("rowp", [1, 4]), ("bc", [1, 512]), ("lora", [128, 512])):
        dr[nm] = P.dram(f"{pre}_{nm}", shp, F32, kind="ExternalInput")
    return dr


MM_DT = BF16
ALPHA = float((2 * 2) ** 0.25)
LN_EPS = 1e-5


def mod_compute(P, C, dr, scratch, pre, need_gt=True):
    sb = lambda shape, nm: P.sb(shape, name=f"{pre}m_{nm}")
    ccol = sb([128, 8], "ccol")
    P.dma(ccol[:], dr["ccol"][:])
    cond = sb([128, 8], "cond")
    P.act(cond[:], ccol[:], AF.Silu)
    condB = sb([128, 8, 128], "condB")
    for k in range(8):
        P.copy(condB[:, k, :], cond[:, k:k + 1].bt([128, 128]), eng="dve")
    abc = sb([128, 48], "abc")
    P.dma(abc[:], dr["adab_col"][:])
    modc = sb([128, 48], "modc")
    gtb = [sb([128, 1024], f"gtb{i}") for i in range(2)] if need_gt else None
    for i, ch in enumerate((2, 5) if need_gt else ()):
        P.dma(gtb[i][:], dr["adab_row"][:, ch * 1024:(ch + 1) * 1024].pb(128))
    BW = 256
    blk = scratch[:, 0:8 * BW].re("p (k f) -> p k f", k=8)
    pc = C.full[0]
    for ch in range(6):
        if ch in (2, 5) and not need_gt:
            continue
        for hf in range(1024 // BW):
            c0 = ch * 1024 + hf * BW
            P.dma(blk, dr["ada_w"][:, c0:c0 + BW].re("(k p) f -> p k f", p=128))
            if ch in (2, 5):
                gi = 0 if ch == 2 else 1
                for k in range(8):
                    P.mm(C.full[1][:, 0:BW], condB[:, k, :], blk[:, k, :], start=(k == 0), stop=(k == 7))
                P.tt(gtb[gi][:, hf * BW:(hf + 1) * BW], gtb[gi][:, hf * BW:(hf + 1) * BW], C.full[1][:, 0:BW], ALU.add)
            else:
                nj = BW // 128
                for j in range(nj):
                    for k in range(8):
                        P.mm(pc[:, j:j + 1], blk[:, k, j * 128:(j + 1) * 128], cond[:, k:k + 1], start=(k == 0), stop=(k == 7))
                cc = ch * 8 + hf * nj
                P.tt(modc[:, cc:cc + nj], pc[:, 0:nj], abc[:, cc:cc + nj], ALU.add)
    for i in range(2 if need_gt else 0):
        P.ts(gtb[i][:], gtb[i][:], 1.0, ALU.add)
    return modc, gtb


def ln_tile(P, xin, xout, gB, bB, st):
    P.reduce(st[:, 0:1], xin, ALU.add)
    P.ts(st[:, 1:2], st[:, 0:1], -1.0 / 1024.0, ALU.mult)
    P.act(xout, xin, AF.Identity, bias=st[:, 1:2])
    P.act(xin, xout, AF.Square, accum=st[:, 2:3])
    P.act(st[:, 3:4], st[:, 2:3], AF.Ln, scale=1.0 / 1024.0, bias=LN_EPS)
    P.act(st[:, 3:4], st[:, 3:4], AF.Exp, scale=-0.5)
    P.stt(xout, xout, st[:, 3:4], gB, ALU.mult, ALU.mult)
    P.tt(xout, xout, bB, ALU.add)


def to_feature_major(P, C, x_tm, hT_dst, sccol, shcol, bank):
    for k in range(8):
        reg = bank[:, (k % 4) * 128:(k % 4 + 1) * 128]
        P.mm(reg, x_tm[:, k * 128:(k + 1) * 128], C.ident)
        P.act(hT_dst[:, k, :], reg, AF.Identity, scale=sccol[:, k:k + 1], bias=shcol[:, k:k + 1])


def prologue_phase(P, C, NTOK, dr, pre="a"):
    sb = lambda shape, nm: P.sb(shape, name=f"{pre}s_{nm}")
    scratch = sb([128, 4096], "scr")
    modc, gtb = mod_compute(P, C, dr, scratch, pre)
    sc1 = sb([128, 8], "sc1")
    P.ts(sc1[:], modc[:, 8:16], 1.0, ALU.add)
    gB, bB = sb([128, 1024], "gB"), sb([128, 1024], "bB")
    P.dma(gB[:], dr["lng"][:].pb(128))
    P.dma(bB[:], dr["lnb"][:].pb(128))
    xt = [sb([128, 1024], f"xt{i}") for i in range(2)]
    xo = [sb([128, 1024], f"xo{i}") for i in range(2)]
    hTs = [sb([128, 8, 128], f"hTs{i}") for i in range(2)]
    st = [sb([128, 4], f"st{i}") for i in range(2)]
    for t in range(NTOK // 128):
        i = t % 2
        P.dma(xt[i][:], dr["x"][t * 128:(t + 1) * 128, :])
        ln_tile(P, xt[i][:], xo[i][:], gB[:], bB[:], st[i])
        P.dma(dr["x0"][t * 128:(t + 1) * 128, :], xo[i][:])
        to_feature_major(P, C, xo[i], hTs[i], sc1, modc[:, 0:8], C.full[t % 2])
        P.dma(dr["hT"][:, t * 128:(t + 1) * 128].re("(k p) t -> p k t", p=128), hTs[i][:])


def post_phase(P, C, NTOK, TB, dr, kind, NFT, NE, emit_next, pre="c", gath=False):
    sb = lambda shape, nm: P.sb(shape, name=f"{pre}s_{nm}")
    NT = TB // 128
    ident = C.ident
    hid = P.sb([128, NFT, TB], MM_DT, name=f"{pre}s_hid")
    NBG = 3
    wgu32 = [sb([128, 2, 8, 128], f"wgu{i}") for i in range(NBG)]
    scr = wgu32[0][:].re("p a b c -> p (a b c)")
    modc, gtb = mod_compute(P, C, dr, scr, pre)
    sc2 = sb([128, 8], "sc2")
    P.ts(sc2[:], modc[:, 32:40], 1.0, ALU.add)
    sh2 = modc[:, 24:32]
    if emit_next:
        nx = {k[3:]: v for k, v in dr.items() if k.startswith("nx_")}
        modn, _ = mod_compute(P, C, nx, scr, pre + "n", need_gt=False)
        sc1n = sb([128, 8], "sc1n")
        P.ts(sc1n[:], modn[:, 8:16], 1.0, ALU.add)
    lnr = [sb([128, 1024], f"lnr{i}") for i in range(4)]
    for i, nm in enumerate(("ln1g", "ln1b", "ln2g", "ln2b")):
        P.dma(lnr[i][:], dr[nm][:].pb(128))
    LP = MM_DT != F32
    sig, tmp = sb([128, TB], "sig"), sb([128, TB], "tmp")
    sg, tmp2 = sb([128, TB], "sg"), sb([128, 1024], "tmp2")
    sbm = lambda shape, nm: P.sb(shape, MM_DT, name=f"{pre}s_{nm}")
    cast_i = [0]

    def cast(dst, src):
        cast_i[0] += 1
        P.copy(dst, src, eng=("dve" if cast_i[0] % 3 else "act"))
    wo = sbm([128, 8, 1024], "wo")
    if LP:
        for k in range(8):
            P.dma(tmp2[:], dr["wo"][k * 128:(k + 1) * 128, :])
            cast(wo[:, k, :], tmp2[:])
    else:
        P.dma(wo[:], dr["wo"][:].re("(k p) f -> p k f", p=128))
    hT32, yT32 = sb([128, 8, TB], "hT"), sb([128, 12, TB], "yT")
    hT, yT = (sbm([128, 8, TB], "hTb"), sbm([128, 12, TB], "yTb")) if LP else (hT32, yT32)
    mg = sbm([128, 8, TB], "mg")
    xr, x1 = sb([128, NT, 1024], "xr"), sb([128, NT, 1024], "x1")
    h2T32 = sb([128, 8, TB], "h2T") if (kind == "moe" or not LP) else None
    h2T = sbm([128, 8, TB], "h2Tb") if LP else h2T32
    wgj32, wbj32 = sb([128, 8, 128], "wgj"), sb([128, 12, 128], "wbj")
    wgj, wbj = (sbm([128, 8, 128], "wgjb"), sbm([128, 12, 128], "wbjb")) if LP else (wgj32, wbj32)
    NBUF = 2 if LP else 3
    wd32 = [sb([128, 1024], f"wd{i}") for i in range(NBUF)]
    wgu = [sbm([128, 2, 8, 128], f"wgub{i}") for i in range(2)] if LP else wgu32
    wd = [sbm([128, 1024], f"wdb{i}") for i in range(2)] if LP else wd32
    st = sb([128, 8], "st")
    yacc = sb([128, NT, 1024], "yacc")
    if kind == "moe":
        rt = sb([128, 8, 8], "rt")
        P.dma(rt[:], dr["router"][:].re("(k p) e -> p k e", p=128))
        rb = sb([128, 8], "rb")
        P.dma(rb[:], dr["router_b"][:].pb(128))
        lg, eq1, lg2, eq2, gts = (sb([128, NT, 8], n_) for n_ in ("lg", "eq1", "lg2", "eq2", "gts"))
        mst = sb([128, NT, 8], "mst")
    self_banks = C.bankT[:2 * NT]
    if gath:
        yg, yq = dr["yT"], dr["yTq"]
        ygf = yg.t.ap().rearrange("c i r w -> (c i r) w")
        for i in range(4):
            def dyn(val, i=i):
                return ygf[i * 384:(i + 1) * 384, bass.ds(val * NTOK, NTOK)]
            P.dma(yq[i * 384:(i + 1) * 384, :], V(ygf[i * 384:(i + 1) * 384, 0:NTOK], [yg.tok]), dyn=dyn)
    for b0 in range(NTOK // TB):
        ts_ = slice(b0 * TB, (b0 + 1) * TB)
        P.dma(hT32[:], dr["hT"][:, ts_].re("(k p) t -> p k t", p=128))
        if LP:
            cast(hT[:], hT32[:])
        if not gath:
            P.dma(yT32[:], dr["yT"][:, ts_].re("(k p) t -> p k t", p=128))
            if LP:
                cast(yT[:], yT32[:])
        else:
            yq = dr["yTq"]
            for r in range(3):
                for pc in range(4):
                    c_ = r * 4 + pc
                    P.dma(yT32[pc * 32:(pc + 1) * 32, r * 4:(r + 1) * 4, :], yq[c_ * 128:(c_ + 1) * 128, ts_].re("(i rr) t -> rr i t", i=4))
            if LP:
                cast(yT[:], yT32[:])
        for t in range(NT):
            P.dma(xr[:, t, :], dr["x"][b0 * TB + t * 128:b0 * TB + (t + 1) * 128, :])
        for j in range(8):
            P.dma(wbj32[:], dr["wbj"][j])
            if LP:
                cast(wbj[:], wbj32[:])
            for r in range(3):
                pg, pp = C.full[0], C.full[1]
                P.dma(wgj32[:], dr["wgj"][j][:, r, :, :])
                if LP:
                    cast(wgj[:], wgj32[:])
                for k in range(8):
                    P.mm(pg[:, 0:TB], wgj[:, k, :], hT[:, k, :], start=(k == 0), stop=(k == 7))
                P.act(sig[:], pg[:, 0:TB], AF.Sigmoid)
                for k in range(4):
                    P.mm(pp[:, 0:TB], wbj[:, r * 4 + k, :], yT[:, r * 4 + k, :], start=(k == 0), stop=(k == 3))
                if r == 0:
                    P.tt(mg[:, j, :], pp[:, 0:TB], sig[:], ALU.mult)
                else:
                    P.tt(tmp[:], pp[:, 0:TB], sig[:], ALU.mult)
                    P.tt(mg[:, j, :], mg[:, j, :], tmp[:], ALU.add)
        for t in range(NT):
            for hf in range(2):
                po = C.full[hf]
                for k in range(8):
                    P.mm(po[:, 0:512], mg[:, k, t * 128:(t + 1) * 128], wo[:, k, hf * 512:(hf + 1) * 512], start=(k == 0), stop=(k == 7))
                P.tt(tmp2[:, hf * 512:(hf + 1) * 512], po[:, 0:512], gtb[0][:, hf * 512:(hf + 1) * 512], ALU.mult)
            P.stt(tmp2[:], xr[:, t, :], ALPHA, tmp2[:], ALU.mult, ALU.add)
            ln_tile(P, tmp2[:], x1[:, t, :], lnr[0][:], lnr[1][:], st)
            for k in range(8):
                reg = C.q[(k % 4) * 4][:, :]
                P.mm(reg, x1[:, t, k * 128:(k + 1) * 128], ident)
                if h2T32 is not None and LP:
                    P.act(h2T32[:, k, t * 128:(t + 1) * 128], reg, AF.Identity, scale=sc2[:, k:k + 1], bias=sh2[:, k:k + 1])
                    P.copy(h2T[:, k, t * 128:(t + 1) * 128], h2T32[:, k, t * 128:(t + 1) * 128], eng="dve")
                else:
                    P.act(h2T[:, k, t * 128:(t + 1) * 128], reg, AF.Identity, scale=sc2[:, k:k + 1], bias=sh2[:, k:k + 1])

        def swiglu(wgu_d, wd_d, consume):
            for i in range(NFT):
                w_ = wgu[i % 2] if LP else wgu32[i % NBG]
                P.dma(wgu32[i % NBG][:, 0, :, :], wgu_d[i][:, 0, :, :])
                P.dma(wgu32[i % NBG][:, 1, :, :], wgu_d[i][:, 1, :, :])
                if LP:
                    cast(w_[:, 0, :, :], wgu32[i % NBG][:, 0, :, :])
                    cast(w_[:, 1, :, :], wgu32[i % NBG][:, 1, :, :])
                pg, pu = C.full[0], C.full[1]
                for k in range(8):
                    P.mm(pg[:, 0:TB], w_[:, 0, k, :], h2T[:, k, :], start=(k == 0), stop=(k == 7))
                P.act(sg[:], pg[:, 0:TB], AF.Silu)
                for k in range(8):
                    P.mm(pu[:, 0:TB], w_[:, 1, k, :], h2T[:, k, :], start=(k == 0), stop=(k == 7))
                P.tt(hid[:, i, :], pu[:, 0:TB], sg[:], ALU.mult)
            for i in range(NFT):
                w2 = wd[i % 2] if LP else wd32[i % NBUF]
                P.dma(wd32[i % NBUF][:, 0:512], wd_d[i * 128:(i + 1) * 128, 0:512])
                P.dma(wd32[i % NBUF][:, 512:1024], wd_d[i * 128:(i + 1) * 128, 512:1024])
                if LP:
                    cast(w2[:], wd32[i % NBUF][:])
                for t in range(NT):
                    for hf in range(2):
                        P.mm(self_banks[2 * t + hf][:, 0:512], hid[:, i, t * 128:(t + 1) * 128], w2[:, hf * 512:(hf + 1) * 512],
                             start=(i == 0), stop=(i == NFT - 1))
            for t in range(NT):
                for hf in range(2):
                    consume(t, hf, self_banks[2 * t + hf][:, 0:512])

        if kind == "dense":
            def consume(t, hf, ps_):
                P.tt(yacc[:, t, hf * 512:(hf + 1) * 512], ps_, gtb[1][:, hf * 512:(hf + 1) * 512], ALU.mult)
            swiglu(dr["wgu"], dr["wd"], consume)
        else:
            for t in range(NT):
                pl = C.full[0]
                for k in range(8):
                    P.mm(pl[:, 0:8], h2T32[:, k, t * 128:(t + 1) * 128], rt[:, k, :], start=(k == 0), stop=(k == 7))
                P.tt(lg[:, t, :], pl[:, 0:8], rb[:], ALU.add)
                P.reduce(mst[:, t, 0:1], lg[:, t, :], ALU.max)
                P.ts(eq1[:, t, :], lg[:, t, :], mst[:, t, 0:1], ALU.is_equal)
                P.stt(lg2[:, t, :], eq1[:, t, :], -1e30, lg[:, t, :], ALU.mult, ALU.add)
                P.reduce(mst[:, t, 1:2], lg2[:, t, :], ALU.max)
                P.ts(eq2[:, t, :], lg2[:, t, :], mst[:, t, 1:2], ALU.is_equal)
                P.tt(mst[:, t, 2:3], mst[:, t, 1:2], mst[:, t, 0:1], ALU.subtract)
                P.act(mst[:, t, 3:4], mst[:, t, 2:3], AF.Exp)
                P.ts(mst[:, t, 4:5], mst[:, t, 3:4], 1.0, ALU.add)
                P.recip(mst[:, t, 5:6], mst[:, t, 4:5])
                P.tt(mst[:, t, 6:7], mst[:, t, 3:4], mst[:, t, 5:6], ALU.mult)
                P.ts(gts[:, t, :], eq1[:, t, :], mst[:, t, 5:6], ALU.mult)
                P.stt(gts[:, t, :], eq2[:, t, :], mst[:, t, 6:7], gts[:, t, :], ALU.mult, ALU.add)
            import os as _os
            for e in range(int(_os.environ.get('DBGNE', NE))):
                def consume(t, hf, ps_, e=e):
                    sl = yacc[:, t, hf * 512:(hf + 1) * 512]
                    if e == 0:
                        P.ts(sl, ps_, gts[:, t, e:e + 1], ALU.mult)
                    else:
                        P.ts(tmp2[:, 0:512], ps_, gts[:, t, e:e + 1], ALU.mult)
                        P.tt(sl, sl, tmp2[:, 0:512], ALU.add)
                swiglu(dr["wgu"][e], dr["wd"][e], consume)
            for t in range(NT):
                P.tt(yacc[:, t, :], yacc[:, t, :], gtb[1][:], ALU.mult)
        for t in range(NT):
            P.stt(tmp2[:], x1[:, t, :], ALPHA, yacc[:, t, :], ALU.mult, ALU.add)
            ln_tile(P, tmp2[:], xr[:, t, :], lnr[2][:], lnr[3][:], st)
            P.dma(dr["xo"][b0 * TB + t * 128:b0 * TB + (t + 1) * 128, :], xr[:, t, :])
            if emit_next:
                for k in range(8):
                    reg = C.q[(k % 4) * 4][:, :]
                    P.mm(reg, xr[:, t, k * 128:(k + 1) * 128], ident)
                    P.act(hT32[:, k, t * 128:(t + 1) * 128], reg, AF.Identity, scale=sc1n[:, k:k + 1], bias=modn[:, k:k + 1])
        if emit_next:
            P.dma(dr["hTn"][:, ts_].re("(k p) t -> p k t", p=128), hT32[:])


def prep_mod_inputs(inp, l, b):
    f = np.float32
    return dict(ccol=np.ascontiguousarray(inp["c"][b].reshape(8, 128).T.astype(f)),
                ada_w=inp["ada_w"][l],
                adab_col=np.ascontiguousarray(inp["ada_b"][l].reshape(48, 128).T.astype(f)),
                adab_row=inp["ada_b"][l].reshape(1, 6144))


def prep_post_weights(inp, l, NFT=None, NE=None):
    f = np.float32
    d = {}
    wg = inp["w_in"][l][:, GATE_OFF:GATE_OFF + 3072]
    d["wgj"] = np.ascontiguousarray(wg.reshape(8, 128, 3, 8, 128).transpose(3, 1, 2, 0, 4))
    wb = inp["w_branch"][l].reshape(12, 128, 8, 128)
    d["wbj"] = np.ascontiguousarray(wb.transpose(2, 1, 0, 3))
    d["wo"] = inp["w_o"][l]
    for i, nm in enumerate(("ln1_g", "ln1_b", "ln2_g", "ln2_b")):
        d[("ln1g", "ln1b", "ln2g", "ln2b")[i]] = inp[nm][l].reshape(1, 1024)

    def gu(wg_, wu_, nft):
        a = wg_[:, :nft * 128].reshape(8, 128, nft, 128).transpose(2, 1, 0, 3)
        b = wu_[:, :nft * 128].reshape(8, 128, nft, 128).transpose(2, 1, 0, 3)
        return np.ascontiguousarray(np.stack([a, b], axis=2))
    if l % 2 == 0:
        i = l // 2
        nft = NFT or 22
        d["wgu"] = gu(inp["ffn_w_gate"][i], inp["ffn_w_up"][i], nft)
        d["wd"] = np.ascontiguousarray(inp["ffn_w_down"][i][:nft * 128])
    else:
        i = l // 2
        nft = NFT or 28
        ne = NE or 8
        d["wgu"] = np.stack([gu(inp["moe_w_gate"][i][e], inp["moe_w_up"][i][e], nft) for e in range(ne)])
        d["wd"] = np.ascontiguousarray(inp["moe_w_down"][i][:ne, :nft * 128])
        d["router"] = np.ascontiguousarray(inp["moe_router"][i][:, :8])
        d["router_b"] = inp["moe_router_b"][i].reshape(1, 8)
    return d


def declare_inputs(P, arrays, pre):
    return {k: P.dram(f"{pre}_{k}", list(v.shape), F32, kind="ExternalInput") for k, v in arrays.items()}


NB, NQ = 2, 4
SEG = 256
TBLK = 256
GROUPS = [[0, 1, 2, 3], [4, 5, 6, 7]]


def _new_prog():
    nc = bass.Bass("TRN2", target_bir_lowering=False)
    P = Prog(nc)
    consts = P.dram("consts", [128, 896], F32, kind="ExternalInput")
    C = setup_common(P, consts)
    return nc, P, C


def fused_inputs(inp, TSEQ, nft_d=22, nft_m=28):
    NTOK = TSEQ // NQ
    consts = host_consts()
    pws = [prep_post_weights(inp, 0, nft_d), prep_post_weights(inp, 1, nft_m)]
    maps = []
    for b in range(NB):
        mods = [prep_mod_inputs(inp, l, b) for l in range(2)]
        for q in range(NQ):
            m = {"consts": consts, "qidx": np.array([[q]], np.int32)}
            m.update({"a_" + k: v for k, v in mods[0].items()})
            m["a_x"] = np.ascontiguousarray(inp["x"][b, q * NTOK:(q + 1) * NTOK])
            m["a_lng"] = inp["ln_in_g"].reshape(1, 1024)
            m["a_lnb"] = inp["ln_in_b"].reshape(1, 1024)
            for l in range(2):
                m.update({f"m{l}_" + k: v for k, v in prep_mixer_inputs(inp, l, q).items()})
                m.update({f"c{l}_" + k: v for k, v in pws[l].items()})
                m.update({f"c{l}_" + k: v for k, v in mods[l].items()})
            m.update({"c0_nx_" + k: v for k, v in mods[1].items()})
            maps.append(m)
    return maps


def build_fused(maps0, TSEQ, nft_d=22, nft_m=28):
    NTOK = TSEQ // NQ
    nc, P, C = _new_prog()
    ext = {k: P.dram(k, list(v.shape), mybir.dt.int32 if v.dtype == np.int32 else F32, kind="ExternalInput")
           for k, v in maps0.items() if k != "consts"}
    sub = lambda pre: {k[len(pre):]: v for k, v in ext.items() if k.startswith(pre)}
    out = P.dram("out", [NTOK, 1024], F32, kind="ExternalOutput")
    x0 = P.dram("i_x0", [NTOK, 1024])
    x1 = P.dram("i_x1", [NTOK, 1024])
    hTs = [P.dram(f"i_hTs{l}", [1024, NTOK]) for l in range(2)]
    hTg = [P.dram(f"i_hTg{l}", [8, 4, 128, NTOK]) for l in range(2)]
    yT = [P.dram(f"i_yT{l}", [384, TSEQ]) for l in range(2)]
    yTg = [P.dram(f"i_yTg{l}", [12, 4, 32, TSEQ]) for l in range(2)]
    vf = P.dram("i_vf", [128, TSEQ])
    yTq = [P.dram(f"i_yTq{l}", [4 * 384, NTOK]) for l in range(2)]
    P.load_dyn(ext["qidx"][0:1, 0:1])
    with P.scope():
        dr = sub("a_")
        dr.update(x0=x0, hT=hTs[0])
        prologue_phase(P, C, NTOK, dr)
    xin = x0
    import os as _os
    stop = int(_os.environ.get("FUSE_STOP", 99))
    for l in range(2):
        if stop <= 1 + 4 * l:
            break
        P.all_gather_rows(hTg[l], hTs[l], 128, GROUPS)
        if stop <= 2 + 4 * l:
            break
        with P.scope():
            dr = sub(f"m{l}_")
            dr.update(hT=hTg[l], yT=yT[l])
            dr["vfirst_in" if l else "vfirst_out"] = vf
            mixer_phase(P, C, TSEQ, SEG, dr, l > 0, pre=f"m{l}", gath=NTOK)
        if stop <= 3 + 4 * l:
            break
        P.all_gather_rows(yTg[l], yT[l], 32 if TSEQ * 32 * 4 <= (1 << 20) else 16, GROUPS)
        if stop <= 4 + 4 * l:
            break
        with P.scope():
            dr = sub(f"c{l}_")
            dr.update(x=xin, hT=hTs[l], yT=yTg[l], yTq=yTq[l], xo=(x1 if l == 0 else out))
            if l == 0:
                dr["hTn"] = hTs[1]
            post_phase(P, C, NTOK, TBLK, dr, "dense" if l == 0 else "moe", nft_d if l == 0 else nft_m, 8, l == 0, pre=f"c{l}", gath=True)
        xin = x1
    P.emit()
    return nc, P


def kernel(**inputs):
    inp = {k: np.asarray(v) for k, v in inputs.items()}
    TSEQ = inp["x"].shape[1]
    NTOK = TSEQ // NQ
    maps = fused_inputs(inp, TSEQ)
    nc, P = build_fused(maps[0], TSEQ)
    res = run_bass_kernel_spmd(nc, maps, core_ids=list(range(NB * NQ))).results
    out = np.zeros((NB, TSEQ, 1024), np.float32)
    for ci in range(NB * NQ):
        b, q = divmod(ci, NQ)
        out[b, q * NTOK:(q + 1) * NTOK] = res[ci]["out"]
    return out
```

```python
import contextlib
import numpy as np
import concourse.bass as bass
import concourse.mybir as mybir
from concourse.bass_utils import run_bass_kernel_spmd

F32 = mybir.dt.float32
BF16 = mybir.dt.bfloat16
AF = mybir.ActivationFunctionType
ALU = mybir.AluOpType
AX = mybir.AxisListType

ENGS = ("pe", "act", "dve", "pool", "sp")


class Tok:
    __slots__ = ("w", "r", "dsem", "dcnt", "name", "isout", "isdram", "wl", "excl")

    def __init__(self, name=""):
        self.excl = False
        self.isdram = False
        self.wl = []
        self.w = None
        self.r = []
        self.dsem = None
        self.dcnt = 0
        self.name = name
        self.isout = False


class V:
    __slots__ = ("ap", "toks")

    def __init__(self, ap, toks):
        self.ap = ap
        self.toks = toks

    def __getitem__(self, idx):
        return V(self.ap[idx], self.toks)

    def re(self, pat, **kw):
        return V(self.ap.rearrange(pat, **kw), self.toks)

    def pb(self, n):
        return V(self.ap.partition_broadcast(n), self.toks)

    def bt(self, shape):
        return V(self.ap.broadcast_to(list(shape)), self.toks)

    def tb(self, shape):
        return V(self.ap.to_broadcast(list(shape)), self.toks)


class T:
    def __init__(self, handle, name):
        self.t = handle
        self.tok = Tok(name)

    def __getitem__(self, idx):
        return V(self.t[idx], [self.tok])

    def ap(self):
        return V(self.t.ap() if hasattr(self.t, "ap") and callable(getattr(self.t, "ap")) else self.t[:], [self.tok])


class Tsub:
    def __init__(self, base_ap, name, tok=None):
        self.base = base_ap
        self.tok = tok if tok is not None else Tok(name)

    def __getitem__(self, idx):
        return V(self.base[idx], [self.tok])


class Op:
    __slots__ = ("eng", "fn", "deps", "isdma", "dtok", "handle", "awaited", "idx", "outdram", "incval", "epoch")

    def __init__(self, eng, fn, isdma=False, dtok=None):
        self.eng = eng
        self.fn = fn
        self.deps = []
        self.isdma = isdma
        self.dtok = dtok
        self.handle = None
        self.awaited = False
        self.outdram = False
        self.incval = 16


def _nofn(eng):
    return None


def _need(op, d, raw):
    if (not op.isdma) and (not d.isdma) and op.eng == "pe" and d.eng == "pe" and not raw:
        return False
    return True


def _aps(x):
    return x.ap if isinstance(x, V) else x


class Prog:
    def __init__(self, nc):
        self.nc = nc
        self.es = contextlib.ExitStack()
        self.ops = {e: [] for e in ENGS}
        self.all_ops = []
        self.scopes = [self.es]
        self.dma_last = {}
        self.dynval = None
        self.arena = None
        self.in_scope = False
        self.arena_off = 0
        self.epoch = 0
        self.n = 0
        self.outtoks = []

    def sb(self, shape, dt=F32, name=None):
        self.n += 1
        name = name or f"sb{self.n}"
        if self.in_scope and dt in (F32, BF16):
            p = shape[0]
            n = int(np.prod(shape[1:]))
            nw = n if dt == F32 else (n + 1) // 2
            n2 = (nw + 7) // 8 * 8
            assert self.arena_off + n2 <= self.ARENA, f"arena overflow allocating {name} {shape}: off={self.arena_off}"
            v = self.arena[0:p, self.arena_off:self.arena_off + nw]
            if dt == BF16:
                v = v.bitcast(BF16)[:, 0:n]
            self.arena_off += n2
            if len(shape) == 3:
                v = v.rearrange("p (a b) -> p a b", a=shape[1], b=shape[2])
            elif len(shape) == 4:
                v = v.rearrange("p (a b c) -> p a b c", a=shape[1], b=shape[2], c=shape[3])
            return T(v, name)
        return T(self.es.enter_context(self.nc.sbuf_tensor(name, list(shape), dt)), name)

    def ps(self, shape, dt=F32, name=None):
        self.n += 1
        name = name or f"ps{self.n}"
        t = T(self.es.enter_context(self.nc.psum_tensor(name, list(shape), dt)), name)
        t.tok.excl = True
        return t

    def dram(self, name, shape, dt=F32, kind="Internal"):
        t = T(self.nc.dram_tensor(name, list(shape), dt, kind=kind), name)
        t.tok.isdram = True
        if kind == "ExternalOutput":
            t.tok.isout = True
            self.outtoks.append(t.tok)
        return t

    ARENA = 52000

    @contextlib.contextmanager
    def scope(self):
        if self.arena is None:
            self.arena = self.es.enter_context(self.nc.sbuf_tensor("arena", [128, self.ARENA], F32))
        self.in_scope = True
        self.arena_off = 0
        try:
            yield
        finally:
            self.barrier()
            self.in_scope = False

    def barrier(self):
        deps = []
        for e in ENGS:
            comp = [o for o in self.ops[e] if not o.isdma and o.fn is not _nofn]
            if comp:
                deps.append(comp[-1])
        deps += list(self.dma_last.values())
        for e in ENGS:
            op = Op(e, _nofn)
            op.deps = [(d, True) for d in deps]
            op.epoch = self.epoch
            self.ops[e].append(op)
            self.all_ops.append(op)
        self.epoch += 1

    def load_dyn(self, src):
        ap = src.ap

        def fn(e):
            reg = self.es.enter_context(e.register("dynreg"))
            ins = e.reg_load(reg, ap)
            self.dynval = e.snap(reg, min_val=0, max_val=3)
            return ins
        return self._rec(Op("sp", fn), [src], [])

    def _rec(self, op, reads, writes):
        if getattr(self, "defer", None) is not None:
            self.defer.append((op, list(reads), list(writes)))
            return op
        xr = []
        for v in reads:
            for tk in (v.toks if isinstance(v, V) else [v]):
                if tk.excl:
                    xr.append(tk)
                    continue
                w = tk.w
                if tk.isdram:
                    for w_ in tk.wl:
                        op.deps.append((w_, True))
                elif w is not None:
                    op.deps.append((w, True))
                if not op.isdma:
                    tk.r = [r for r in tk.r if r.isdma or r.eng != op.eng]
                tk.r.append(op)
        wl_ = [(tk, True) for tk in xr]
        for v in writes:
            for tk in (v.toks if isinstance(v, V) else [v]):
                wl_.append((tk, False))
        for tk, israw in wl_:
            if True:
                w = tk.w
                if w is not None and not (w.isdma and op.isdma):
                    op.deps.append((w, israw))
                for r in tk.r:
                    if r is not op:
                        op.deps.append((r, False))
                tk.r = []
                tk.w = op
                if tk.isdram:
                    tk.wl.append(op)
        op.epoch = self.epoch
        op.deps = [(d, raw) for d, raw in op.deps if d.epoch >= self.epoch]
        self.ops[op.eng].append(op)
        self.all_ops.append(op)
        if op.isdma:
            self.dma_last[id(op.dtok)] = op
        return op

    def op(self, eng, fn, reads=(), writes=()):
        return self._rec(Op(eng, fn), reads, writes)

    def begin_defer(self):
        self.defer = []

    def end_defer(self):
        d, self.defer = self.defer, None
        return d

    def merge(self, lists):
        def toks(vs):
            return [tk for v in vs for tk in (v.toks if isinstance(v, V) else [v])]
        touched, written = [], []
        for L in lists:
            t_, w_ = set(), set()
            for op, reads, writes in L:
                for tk in toks(reads):
                    if not tk.isdram:
                        t_.add(id(tk))
                for tk in toks(writes):
                    if not tk.isdram:
                        t_.add(id(tk)); w_.add(id(tk))
            touched.append(t_); written.append(w_)
        for a in range(len(lists)):
            for b in range(len(lists)):
                if a != b:
                    assert not (written[a] & touched[b]), "interleaved streams share a written tile"
        idx = [0] * len(lists)
        tot = [max(1, len(L)) for L in lists]
        while any(idx[i] < len(lists[i]) for i in range(len(lists))):
            i = min((k for k in range(len(lists)) if idx[k] < len(lists[k])), key=lambda k: idx[k] / tot[k])
            op, reads, writes = lists[i][idx[i]]
            idx[i] += 1
            self._rec(op, reads, writes)

    def dma(self, out, in_, q="sp", extra_reads=(), dyn=None):
        dtok = out.toks[0]
        if dtok.isdram:
            dtok = in_.toks[0]
            if dtok.isdram:
                self.n += 1
                dtok = Tok(f"dd{self.n}")
        o, i = out.ap, in_.ap
        if dyn is not None:
            op = Op(q, lambda e: e.dma_start(out=o, in_=dyn(self.dynval)), isdma=True, dtok=dtok)
        else:
            op = Op(q, lambda e: e.dma_start(out=o, in_=i), isdma=True, dtok=dtok)
        op.outdram = out.toks[0].isout
        return self._rec(op, [in_] + list(extra_reads), [out])

    def collective(self, kind, out, in_, groups):
        if getattr(self, "cct", None) is None:
            self.cct = Tok("cc")
        cct = self.cct
        o, i = out.ap, in_.ap
        op = Op("pool", lambda e: e.collective_compute(kind, ALU.bypass, replica_groups=groups, ins=[i], outs=[o]), isdma=True, dtok=cct)
        op.incval = 1
        return self._rec(op, [in_], [out])

    def all_gather_rows(self, dst, src, rc, groups):
        R = src.t.shape[0]
        for c in range(R // rc):
            self.collective("AllGather", dst[c].re("i r w -> (i r) w"), src[c * rc:(c + 1) * rc, :], groups)

    def mm(self, out, lhsT, rhs, start=True, stop=True, extra_reads=()):
        o, l, r = out.ap, lhsT.ap, rhs.ap
        return self.op("pe", lambda e: e.matmul(o, l, r, start=start, stop=stop), [lhsT, rhs] + list(extra_reads), [out])

    def tr(self, out, in_, ident):
        o, i, d = out.ap, in_.ap, ident.ap
        return self.op("pe", lambda e: e.transpose(o, i, d), [in_, ident], [out])

    def act(self, out, in_, func, bias=None, scale=None, accum=None, eng="act"):
        kw = {}
        reads = [in_]
        if bias is not None:
            kw["bias"] = _aps(bias)
            if isinstance(bias, V):
                reads.append(bias)
        if scale is not None:
            kw["scale"] = _aps(scale)
            if isinstance(scale, V):
                reads.append(scale)
        writes = [out]
        if accum is not None:
            kw["accum_out"] = accum.ap
            writes.append(accum)
        o, i = out.ap, in_.ap
        return self.op(eng, lambda e: e.activation(o, i, func, **kw), reads, writes)

    def tt(self, out, in0, in1, op, eng="dve"):
        o, a, b = out.ap, in0.ap, in1.ap
        return self.op(eng, lambda e: e.tensor_tensor(o, a, b, op), [in0, in1], [out])

    def ts(self, out, in0, s1, op0, s2=None, op1=None, eng="dve", accum=None):
        reads = [in0] + [s for s in (s1, s2) if isinstance(s, V)]
        o, a, x1, x2 = out.ap, in0.ap, _aps(s1), _aps(s2)
        writes = [out]
        kw = {}
        if accum is not None:
            kw["accum_out"] = accum.ap
            writes.append(accum)
        if op1 is None:
            return self.op(eng, lambda e: e.tensor_scalar(o, a, x1, None, op0, **kw), reads, writes)
        return self.op(eng, lambda e: e.tensor_scalar(o, a, x1, x2, op0, op1, **kw), reads, writes)

    def stt(self, out, in0, scalar, in1, op0, op1, eng="dve"):
        reads = [in0, in1] + ([scalar] if isinstance(scalar, V) else [])
        o, a, s, b = out.ap, in0.ap, _aps(scalar), in1.ap
        return self.op(eng, lambda e: e.scalar_tensor_tensor(o, a, s, b, op0, op1), reads, [out])

    def copy(self, out, in_, eng="dve"):
        o, i = out.ap, in_.ap
        if eng == "act":
            return self.op(eng, lambda e: e.copy(o, i), [in_], [out])
        return self.op(eng, lambda e: e.tensor_copy(o, i), [in_], [out])

    def memset(self, out, val, eng="dve"):
        o = out.ap
        return self.op(eng, lambda e: e.memset(o, val), [], [out])

    def scan(self, out, d0, d1, init, op0, op1, eng="dve"):
        o, a, b = out.ap, d0.ap, d1.ap
        ini = _aps(init)
        reads = [d0, d1] + ([init] if isinstance(init, V) else [])
        return self.op(eng, lambda e: e.tensor_tensor_scan(o, a, b, ini, op0, op1), reads, [out])

    def recip(self, out, in_):
        o, i = out.ap, in_.ap
        return self.op("dve", lambda e: e.reciprocal(o, i), [in_], [out])

    def reduce(self, out, in_, op, axis=AX.X, eng="dve"):
        o, i = out.ap, in_.ap
        return self.op(eng, lambda e: e.tensor_reduce(o, i, axis, op), [in_], [out])

    def emit(self):
        nc = self.nc
        for e in ENGS:
            for op in self.ops[e]:
                for d, raw in op.deps:
                    if _need(op, d, raw):
                        d.awaited = True
        esem = {e: self.es.enter_context(nc.semaphore(f"s_{e}")) for e in ENGS if e != "sp" or True}
        cnt = {e: 0 for e in ENGS}
        dma_toks = []
        for op in self.all_ops:
            e = op.eng
            if True:
                if op.isdma:
                    tk = op.dtok
                    if tk.dsem is None:
                        tk.dsem = self.es.enter_context(nc.semaphore(f"d_{tk.name}"))
                        dma_toks.append(tk)
                    tk.dcnt += 1
                    op.handle = (tk.dsem, op.incval * tk.dcnt)
                elif op.awaited:
                    cnt[e] += 1
                    op.handle = (esem[e], cnt[e])
        self.stats = dict(n_ops={e: len(self.ops[e]) for e in ENGS}, n_inc=dict(cnt), n_dsem=len(dma_toks))
        fw = {}
        for op in self.all_ops:
            if op.isdma and any(True for _ in [0]) and getattr(op, "outdram", False):
                sem, val = op.handle
                fw[id(sem)] = (sem, max(val, fw.get(id(sem), (sem, 0))[1]))
        final_waits = list(fw.values())

        def run(e, eng):
            known = {}
            for op in self.ops[e]:
                need = {}
                for d, raw in op.deps:
                    if not _need(op, d, raw):
                        continue
                    sem, val = d.handle
                    k = id(sem)
                    if known.get(k, 0) >= val:
                        continue
                    if k not in need or need[k][1] < val:
                        need[k] = (sem, val)
                for k, (sem, val) in need.items():
                    known[k] = val
                    eng.wait_ge(sem, val)
                ins = op.fn(eng)
                if ins is None:
                    continue
                if op.isdma or op.awaited:
                    sem, val = op.handle
                    ins.then_inc(sem, op.incval if op.isdma else 1)
            if e == "sp":
                for sem, val in final_waits:
                    eng.wait_ge(sem, val)

        with nc.Block() as block:
            @block.tensor
            def _(eng):
                run("pe", eng)

            @block.scalar
            def _(eng):
                run("act", eng)

            @block.vector
            def _(eng):
                run("dve", eng)

            @block.gpsimd
            def _(eng):
                run("pool", eng)

            @block.sync
            def _(eng):
                run("sp", eng)
        self.es.close()


NEG = -30000.0
DM = 1024
KT = 8
NF0 = 1280
F_GQ, F_GK, F_GV, F_GG, F_MQ, F_MK, F_MG = 0, 128, 256, 384, 448, 512, 576
F_RW = 640
F_XW, F_XA, F_XG, F_XV = 1024, 1088, 1152, 1280
NCOLP = 40


def host_consts():
    i = np.arange(128)
    r, c = i[:, None], i[None, :]
    f = np.float32
    parts = [np.eye(128, dtype=f),
             np.where(r < c, 0.0, NEG).astype(f),
             np.where(r <= c, 0.0, NEG).astype(f),
             np.where(c < r, 0.0, NEG).astype(f),
             (r < c).astype(f), (r <= c).astype(f), (c < r).astype(f)]
    return np.concatenate(parts, axis=1)


class Ctx:
    pass


def setup_common(P, consts_dram):
    C = Ctx()
    cst = P.sb([128, 7 * 128], name="cst")
    P.dma(cst[:], consts_dram[:])
    C.cst = cst
    C.ident = cst[:, 0:128]
    C.addT_s, C.addT_i, C.addD_s = cst[:, 128:256], cst[:, 256:384], cst[:, 384:512]
    C.mulT_s, C.mulT_i, C.mulD_s = cst[:, 512:640], cst[:, 640:768], cst[:, 768:896]
    C.ones = P.sb([128, 128], name="ones")
    P.memset(C.ones[:], 1.0)
    banks = [P.ps([128, 512], name=f"bank{i}") for i in range(8)]
    C.q = [Tsub(banks[i // 4].t[:, (i % 4) * 128:(i % 4 + 1) * 128], f"q{i}", banks[i // 4].tok) for i in range(16)]
    C.full = [banks[4], banks[5]]
    C.half = [Tsub(banks[6].t[:, 0:256], "h0", banks[6].tok), Tsub(banks[7].t[:, 0:256], "h1", banks[7].tok)]
    C.inv_ps = [C.q[2], C.q[6], C.q[10], C.q[14]]
    C.bankT = banks[0:4]
    C.banks = banks

    def reg(b, c0, nm):
        return Tsub(banks[b].t[:, c0:c0 + 128], nm, banks[b].tok)
    C.r = [reg(4, 256, "r0"), reg(4, 384, "r1"), reg(5, 0, "r2"), reg(5, 128, "r3"), reg(5, 256, "r4"), reg(5, 384, "r5"),
           reg(6, 256, "r6"), reg(6, 384, "r7"), reg(7, 256, "r8"), reg(7, 384, "r9")]
    C.inv_ps_r = [C.r[1], C.r[2], C.r[6], C.r[8]]
    return C


def tri_inverse(P, C, B, A, n, plus, X, XT, Pm, PTm, ps):
    idn = C.ident[0:n, 0:n]
    op = ALU.add if plus else ALU.subtract
    P.tt(X[0][0:n, 0:n], idn, B, op)
    P.tt(XT[0][0:n, 0:n], idn, A, op, eng="dve")
    nlev = int(np.log2(n)) - 1
    curP, curPT = B, A
    xi = 0
    for lev in range(nlev):
        last = lev == nlev - 1
        pi = lev % 2
        P.mm(ps[0][0:n, 0:n], curPT, curP)
        P.copy(Pm[pi][0:n, 0:n], ps[0][0:n, 0:n], eng="act")
        if not last:
            P.mm(ps[1][0:n, 0:n], curP, curPT)
            P.copy(PTm[pi][0:n, 0:n], ps[1][0:n, 0:n], eng="act")
        newP, newPT = Pm[pi][0:n, 0:n], PTm[pi][0:n, 0:n]
        P.mm(ps[2][0:n, 0:n], XT[xi][0:n, 0:n], newP)
        P.tt(X[1 - xi][0:n, 0:n], ps[2][0:n, 0:n], X[xi][0:n, 0:n], ALU.add)
        if not last:
            P.mm(ps[3][0:n, 0:n], newP, XT[xi][0:n, 0:n])
            P.tt(XT[1 - xi][0:n, 0:n], ps[3][0:n, 0:n], XT[xi][0:n, 0:n], ALU.add)
        xi = 1 - xi
        curP, curPT = newP, newPT
    return X[xi][0:n, 0:n]


def mixer_phase(P, C, T, SEG, dr, has_vres, pre="m", gath=None):
    NSEG = T // SEG
    NC128 = SEG // 128
    CR = 128
    NCR = SEG // CR
    NF = NF0 + (32 if has_vres else 0)
    sb = lambda shape, nm: P.sb(shape, name=f"{pre}s_{nm}")
    ident, ones = C.ident, C.ones
    q, half, full = C.q, C.half, C.full
    LP = MM_DT != F32
    wfm = P.sb([128, KT, NF], MM_DT, name=f"{pre}s_wfm")
    wtm = P.sb([128, KT, 384], MM_DT, name=f"{pre}s_wtm")
    wst = [sb([128, NF + 384], f"wst{i}") for i in range(2)] if LP else None
    for k in range(KT):
        if LP:
            w_ = wst[k % 2]
            P.dma(w_[:, 0:NF], dr["wfm"][k * 128:(k + 1) * 128, :])
            P.dma(w_[:, NF:NF + 384], dr["wtm"][k * 128:(k + 1) * 128, :])
            P.copy(wfm[:, k, :], w_[:, 0:NF], eng="dve")
            P.copy(wtm[:, k, :], w_[:, NF:NF + 384], eng="act")
        else:
            P.dma(wfm[:, k, :], dr["wfm"][k * 128:(k + 1) * 128, :])
            P.dma(wtm[:, k, :], dr["wtm"][k * 128:(k + 1) * 128, :])
    colp = sb([128, NCOLP], "colp")
    P.dma(colp[:], dr["colp"][:])
    rowp = sb([1, 4], "rowp")
    P.dma(rowp[:], dr["rowp"][:])
    bc = sb([128, 512], "bc")
    P.dma(bc[:], dr["bc"][:].pb(128))
    lora = sb([128, 512], "lora")
    P.dma(lora[:], dr["lora"][:])
    w2s, a2s, g2s, v2s = lora[0:64, 0:128], lora[0:64, 128:256], lora[:, 256:384], lora[0:32, 384:512]
    sc = sb([1, 8], "sc")
    P.act(sc[:, 0:1], rowp[:, 1:2], AF.Exp)
    P.ts(sc[:, 0:1], sc[:, 0:1], -1.0, ALU.mult)
    P.ts(sc[:, 1:3], rowp[:, 2:4], 1.0 / 15.0, ALU.mult)
    negA, dtb, bi15, bf15 = sc[:, 0:1], rowp[:, 0:1], sc[:, 1:2], sc[:, 2:3]
    P.ts(bc[:, 0:256], bc[:, 0:256], float(np.sqrt(128.0)), ALU.mult)
    for h in range(2):
        P.ts(bc[:, 256 + 128 * h:320 + 128 * h], bc[:, 256 + 128 * h:320 + 128 * h], 8.0, ALU.mult)
    ng_gdn, ng_ml = bc[:, 0:128], bc[:, 128:256]
    rm128 = sb([1, SEG], "rm128")
    P.memset(rm128[:], 1.0)
    P.memset(rm128[:].re("p (n c) -> p n c", c=128)[:, :, 0:1], 0.0)
    rmR = sb([64, SEG], "rmR")
    P.memset(rmR[:], 1.0)
    P.memset(rmR[:].re("p (n c) -> p n c", c=CR)[:, :, 0:1], 0.0)
    Sg = [sb([128, 128], f"Sg{i}") for i in range(2)]
    Sm = [sb([64, 129], f"Sm{i}") for i in range(2)]
    Sr = [[sb([64, 64], f"Sr{h}{i}") for i in range(2)] for h in range(2)]
    for t_ in (Sg[0], Sm[0], Sr[0][0], Sr[1][0]):
        P.memset(t_[:], 0.0)
    sgi, smi, sri = 0, 0, [0, 0]
    hTt = [sb([128, KT, SEG], f"hT{i}") for i in range(2)]
    hTb = [P.sb([128, KT, SEG], MM_DT, name=f"{pre}s_hTb{i}") for i in range(2)] if LP else hTt
    xq, xk, xv = (sb([128, SEG + 3], n_) for n_ in ("xq", "xk", "xv"))
    for t_ in (xq, xk, xv):
        P.memset(t_[:, SEG:SEG + 3], 0.0)
    gq, gk, gv, gsq = (sb([128, SEG], n_) for n_ in ("gq", "gk", "gv", "gsq"))
    gacc = sb([128, SEG], "gacc")
    gg = sb([64, SEG], "gg")
    mq, mk, mg = (sb([64, SEG], n_) for n_ in ("mq", "mk", "mg"))
    zr = [sb([64, SEG + 1], f"zr{h}") for h in range(2)]
    zk = [sb([64, SEG + 1], f"zk{h}") for h in range(2)]
    zv = [sb([64, SEG + 1], f"zv{h}") for h in range(2)]
    zxw, zxa = sb([64, SEG + 1], "zxw"), sb([64, SEG + 1], "zxa")
    zxg = sb([128, SEG + 1], "zxg")
    zxv = sb([32, SEG + 1], "zxv") if has_vres else None
    halo1 = zr + zk + zv + [zxw, zxa, zxg] + ([zxv] if has_vres else [])
    for t_ in halo1:
        P.memset(t_[:, SEG:SEG + 1], 0.0)
    gz = sb([128, NC128, 128], "gz")
    mva = sb([128, NC128, 129], "mva")
    P.memset(mva[:, :, 128:129], 1.0)
    mo = sb([128, NC128, 128], "mo")
    nrow = 16
    rows = [sb([1, SEG], f"row{i}") for i in range(nrow)]
    yst = sb([128, SEG], "yst")
    ystm = sb([128, SEG], "ystm")
    ystr = sb([64, SEG], "ystr")
    def set2(shape, nm):
        return [sb(shape, f"{nm}{i}") for i in range(2)]
    cols, MAT, MA, MQT, Bm, Am, QKm = (set2([128, 128], n_) for n_ in ("cols", "MAT", "MA", "MQT", "Bm", "Am", "QKm"))
    kbg, kd, vb, negw, vnew, osb, ysb, ngz = (set2([128, 129], n_) for n_ in ("kbg", "kd", "vb", "negw", "vnew", "osb", "ysb", "ngz"))
    ssq = set2([128, 4], "ssq")
    X, XT, Pm, PTm = (set2([128, 128], n_) for n_ in ("X", "XT", "Pm", "PTm"))
    X_r, XT_r, Pm_r, PTm_r = (set2([128, 128], n_) for n_ in ("Xr", "XTr", "Pmr", "PTmr"))
    cols_r, Am_r = set2([128, 128], "colsr"), set2([128, 128], "Amr")
    osb_r, ysb_r, vnew_r = (set2([128, 129], n_) for n_ in ("osbr", "ysbr", "vnewr"))
    ssq_r = set2([128, 4], "ssqr")
    rr, rk, rvv = sb([64, SEG], "rr"), sb([64, SEG], "rk"), sb([64, SEG], "rvv")
    xw, xa = sb([64, SEG], "xw"), sb([64, SEG], "xa")
    xg = sb([128, SEG], "xg")
    xvt = sb([32, SEG], "xvt") if has_vres else None
    tmpd = sb([128, SEG], "tmpd")
    lw, logw, av, kkv, k2v, E1, E2, E3, tend, behat, khat, beend, kend, prod = (
        sb([64, SEG], n_) for n_ in ("lw", "logw", "av", "kkv", "k2v", "E1", "E2", "E3", "tend", "behat", "khat", "beend", "kend", "prod"))
    AR = sb([64, NCR, 2, CR], "AR")
    vfs = sb([64, SEG], "vfs") if has_vres else None
    GM, TK = set2([128, 512], "GM"), set2([128, 256], "TK")
    wu, lakv, usb = set2([64, 128], "wu"), set2([128, 64], "lakv"), set2([128, 64], "usb")
    hT_d = dr["hT"]

    for sg in range(NSEG):
        t0 = sg * SEG
        hb = hTt[sg % 2]
        if gath is None:
            P.dma(hb[:], hT_d[:, t0:t0 + SEG].re("(k p) t -> p k t", p=128))
        else:
            rk_, c0_ = t0 // gath, t0 % gath
            for k in range(KT):
                P.dma(hb[:, k, :], hT_d[k][rk_][:, c0_:c0_ + SEG])
        if LP:
            P.copy(hTb[sg % 2][:, 0:4, :], hb[:, 0:4, :], eng="dve")
            P.copy(hTb[sg % 2][:, 4:8, :], hb[:, 4:8, :], eng="act")
            hb = hTb[sg % 2]
        for t_ in (xq, xk, xv):
            P.copy(t_[:, 0:3], t_[:, SEG:SEG + 3], eng="dve")
        for t_ in halo1:
            P.copy(t_[:, 0:1], t_[:, SEG:SEG + 1], eng="dve")
        fi = [0]

        def proj_fm(c0, nr, evac):
            bank = full[fi[0] % 2]
            fi[0] += 1
            for k in range(KT):
                P.mm(bank[0:nr, 0:SEG], wfm[:, k, c0:c0 + nr], hb[:, k, :], start=(k == 0), stop=(k == KT - 1))
            evac(bank[0:nr, 0:SEG])

        proj_fm(F_GQ, 128, lambda p_: P.copy(xq[:, 3:], p_, eng="act"))
        proj_fm(F_GK, 128, lambda p_: P.copy(xk[:, 3:], p_, eng="dve"))
        proj_fm(F_GV, 128, lambda p_: P.copy(xv[:, 3:], p_, eng="act"))
        proj_fm(F_GG, 64, lambda p_: P.copy(gg[:], p_, eng="dve"))
        proj_fm(F_MQ, 64, lambda p_: P.copy(mq[:], p_, eng="act"))
        proj_fm(F_MK, 64, lambda p_: P.copy(mk[:], p_, eng="dve"))
        proj_fm(F_MG, 64, lambda p_: P.copy(mg[:], p_, eng="act"))
        for h in range(2):
            proj_fm(F_RW + h * 192, 64, lambda p_, h=h: P.copy(zr[h][:, 1:], p_, eng="dve"))
            proj_fm(F_RW + h * 192 + 64, 64, lambda p_, h=h: P.copy(zk[h][:, 1:], p_, eng="act"))
            proj_fm(F_RW + h * 192 + 128, 64, lambda p_, h=h: P.copy(zv[h][:, 1:], p_, eng="dve"))
        proj_fm(F_XW, 64, lambda p_: P.copy(zxw[:, 1:], p_, eng="act"))
        proj_fm(F_XA, 64, lambda p_: P.copy(zxa[:, 1:], p_, eng="dve"))
        proj_fm(F_XG, 128, lambda p_: P.copy(zxg[:, 1:], p_, eng="act"))
        if has_vres:
            proj_fm(F_XV, 32, lambda p_: P.copy(zxv[:, 1:], p_, eng="dve"))
        for j in range(NC128):
            bank = full[fi[0] % 2]
            fi[0] += 1
            for k in range(KT):
                P.mm(bank[:, 0:384], hb[:, k, j * 128:(j + 1) * 128], wtm[:, k, :], start=(k == 0), stop=(k == KT - 1))
            P.act(gz[:, j, :], bank[:, 0:128], AF.Silu)
            P.copy(mva[:, j, 0:128], bank[:, 128:256], eng="dve")
            P.act(mo[:, j, :], bank[:, 256:384], AF.Sigmoid)

        P.begin_defer()
        for src, dst, cb in ((xq, gq, 0), (xk, gk, 4), (xv, gv, 8)):
            P.ts(gacc[:], src[:, 0:SEG], colp[:, cb:cb + 1], ALU.mult)
            for j in range(1, 4):
                P.stt(gacc[:], src[:, j:j + SEG], colp[:, cb + j:cb + j + 1], gacc[:], ALU.mult, ALU.add)
            P.act(dst[:], gacc[:], AF.Silu)
        (r_gs, r_gc, r_lb, r_lrk, r_lrq, r_A1, r_B1, r_Q, r_ekbg, r_ekd, r_ebeta, r_eq, r_egl, r_t0, r_t1, r_t2) = rows
        P.act(r_t0[:], gg[0:1, :], AF.Exp, bias=dtb)
        P.act(r_t0[:], r_t0[:], AF.Ln, bias=1.0)
        P.ts(r_gs[:], r_t0[:], negA, ALU.mult)
        P.scan(r_gc[:], rm128[:], r_gs[:], 0.0, ALU.mult, ALU.add)
        P.act(r_t1[:], gg[32:33, :], AF.Exp, scale=-1.0)
        P.act(r_t1[:], r_t1[:], AF.Ln, bias=1.0)
        P.ts(r_lb[:], r_t1[:], -1.0, ALU.mult)
        for src, dst, addc in ((gk, r_lrk, 0.0), (gq, r_lrq, float(np.log(128.0 ** -0.5)))):
            P.act(gsq[:], src[:], AF.Square)
            P.mm(C.bankT[3][0:1, 0:SEG], ones[:, 0:1], gsq[:])
            P.act(r_t2[:], C.bankT[3][0:1, 0:SEG], AF.Ln, bias=1e-6)
            P.ts(dst[:], r_t2[:], -0.5, ALU.mult, addc, ALU.add)
        P.tt(r_A1[:], r_lrk[:], r_lb[:], ALU.add)
        P.tt(r_A1[:], r_A1[:], r_gc[:], ALU.add)
        P.tt(r_B1[:], r_lrk[:], r_gc[:], ALU.subtract)
        P.tt(r_Q[:], r_lrq[:], r_gc[:], ALU.add)
        P.act(r_ekbg[:], r_A1[:], AF.Exp)
        P.act(r_eq[:], r_Q[:], AF.Exp)
        P.act(r_ebeta[:], r_lb[:], AF.Exp)
        glast = r_gc[:].re("p (n c) -> p n c", c=128)[:, :, 127:128]
        P.tt(r_t0[:].re("p (n c) -> p n c", c=128), r_B1[:].re("p (n c) -> p n c", c=128), glast.bt([1, NC128, 128]), ALU.add)
        P.act(r_ekd[:], r_t0[:], AF.Exp)
        P.act(r_egl[:, 0:NC128], r_gc[:].re("p (n c) -> p n c", c=128)[:, :, 127], AF.Exp)
        for j in range(NC128):
            ci = j % 2
            cs = slice(j * 128, (j + 1) * 128)
            S_old, S_new = Sg[sgi], Sg[1 - sgi]
            sgi = 1 - sgi
            for x_, r_ in enumerate((r_ekbg, r_ekd, r_ebeta, r_eq)):
                P.mm(q[0][:, x_:x_ + 1], r_[:, cs], ones[0:1, 0:1])
            P.mm(q[0][:, 4:5], ones[0:1, :], r_egl[:, j:j + 1])
            P.copy(cols[ci][:, 0:5], q[0][:, 0:5], eng="act")
            c_kbg, c_kd, c_beta, c_eq, c_gl = (cols[ci][:, x_:x_ + 1] for x_ in range(5))
            for ps_, lrow, rrow, msk, dst in ((q[1], r_B1, r_A1, C.addT_s, MAT[ci]), (q[2], r_A1, r_B1, C.addD_s, MA[ci]),
                                              (q[3], r_B1, r_Q, C.addT_i, MQT[ci])):
                P.mm(ps_[:, :], lrow[:, cs], ones[0:1, :], start=True, stop=False)
                P.mm(ps_[:, :], ones[0:1, :], rrow[:, cs], start=False, stop=False)
                P.mm(ps_[:, :], ident, msk, start=False, stop=True)
                P.act(dst[:], ps_[:, :], AF.Exp)
            P.mm(q[4][:, :], gk[:, cs], gk[:, cs])
            P.mm(q[5][:, :], gk[:, cs], gq[:, cs])
            P.tt(Bm[ci][:], q[4][:, :], MAT[ci][:], ALU.mult)
            P.tt(Am[ci][:], q[4][:, :], MA[ci][:], ALU.mult)
            P.tt(QKm[ci][:], q[5][:, :], MQT[ci][:], ALU.mult)
            P.mm(q[6][:, :], gk[:, cs], ident)
            P.mm(q[7][:, :], gv[:, cs], ident)
            P.act(kbg[ci][:, 0:128], q[6][:, :], AF.Identity, scale=c_kbg)
            P.ts(kd[ci][:, 0:128], q[6][:, :], c_kd, ALU.mult)
            P.act(vb[ci][:, 0:128], q[7][:, :], AF.Identity, scale=c_beta)
            R = tri_inverse(P, C, Bm[ci][:], Am[ci][:], 128, False, X, XT, Pm, PTm, C.inv_ps)
            P.mm(q[13][:, :], kbg[ci][:, 0:128], R)
            P.act(negw[ci][:, 0:128], q[13][:, :], AF.Identity, scale=-1.0)
            P.mm(q[12][:, :], R, vb[ci][:, 0:128], start=True, stop=False)
            P.mm(q[12][:, :], negw[ci][:, 0:128], S_old[:], start=False, stop=True)
            P.copy(vnew[ci][:, 0:128], q[12][:, :], eng="act")
            P.mm(q[14][:, :], gq[:, cs], S_old[:])
            P.mm(q[15][:, :], QKm[ci][:], vnew[ci][:, 0:128])
            P.mm(q[1][:, :], kd[ci][:, 0:128], vnew[ci][:, 0:128])
            P.stt(S_new[:], S_old[:], c_gl, q[1][:, :], ALU.mult, ALU.add)
            P.act(osb[ci][:, 0:128], q[14][:, :], AF.Identity, scale=c_eq)
            P.tt(osb[ci][:, 0:128], osb[ci][:, 0:128], q[15][:, :], ALU.add)
            P.act(ysb[ci][:, 0:128], osb[ci][:, 0:128], AF.Square, accum=ssq[ci][:, 0:1])
            P.act(ssq[ci][:, 1:2], ssq[ci][:, 0:1], AF.Ln, bias=128.0 * 1e-6)
            P.act(ssq[ci][:, 1:2], ssq[ci][:, 1:2], AF.Exp, scale=-0.5)
            P.tt(ngz[ci][:, 0:128], gz[:, j, :], ng_gdn, ALU.mult, eng="dve")
            P.stt(ysb[ci][:, 0:128], osb[ci][:, 0:128], ssq[ci][:, 1:2], ngz[ci][:, 0:128], ALU.mult, ALU.mult)
            P.mm(q[2][:, :], ysb[ci][:, 0:128], ident)
            P.copy(yst[:, cs], q[2][:, :], eng="act")
        P.dma(dr["yT"][0:128, t0:t0 + SEG], yst[:])

        (r_li, r_lf, r_F, r_Ds, r_ee, r_ekw, r_egl2, r_u0, r_u1) = rows[0:9]
        P.act(r_u0[:], mg[0:1, :], AF.Tanh, scale=1.0 / 15.0, bias=bi15)
        P.ts(r_li[:], r_u0[:], 15.0, ALU.mult)
        P.act(r_u1[:], mg[32:33, :], AF.Tanh, scale=1.0 / 15.0, bias=bf15)
        P.act(r_u1[:], r_u1[:], AF.Exp, scale=-15.0)
        P.act(r_u1[:], r_u1[:], AF.Ln, bias=1.0)
        P.ts(r_lf[:], r_u1[:], -1.0, ALU.mult)
        P.scan(r_F[:], rm128[:], r_lf[:], 0.0, ALU.mult, ALU.add)
        lsc = float(np.log(64.0 ** -0.5))
        P.stt(r_Ds[:], r_li[:], lsc, r_F[:], ALU.add, ALU.subtract)
        P.act(r_ee[:], r_F[:], AF.Exp)
        Flast = r_F[:].re("p (n c) -> p n c", c=128)[:, :, 127:128]
        P.tt(r_u0[:].re("p (n c) -> p n c", c=128), r_Ds[:].re("p (n c) -> p n c", c=128), Flast.bt([1, NC128, 128]), ALU.add)
        P.act(r_ekw[:], r_u0[:], AF.Exp)
        P.act(r_egl2[:, 0:NC128], r_F[:].re("p (n c) -> p n c", c=128)[:, :, 127], AF.Exp)
        for j in range(NC128):
            ci = j % 2
            cs = slice(j * 128, (j + 1) * 128)
            S_old, S_new = Sm[smi], Sm[1 - smi]
            smi = 1 - smi
            P.mm(q[0][:, 0:1], r_ee[:, cs], ones[0:1, 0:1])
            P.mm(q[0][:, 1:2], r_ekw[:, cs], ones[0:1, 0:1])
            P.mm(q[0][:, 2:3], ones[0:1, :], r_egl2[:, j:j + 1])
            P.copy(cols[ci][:, 0:3], q[0][:, 0:3], eng="act")
            c_e, c_kw, c_gl = (cols[ci][:, x_:x_ + 1] for x_ in range(3))
            P.mm(q[1][:, :], r_Ds[:, cs], ones[0:1, :], start=True, stop=False)
            P.mm(q[1][:, :], ones[0:1, :], r_F[:, cs], start=False, stop=False)
            P.mm(q[1][:, :], ident, C.addT_i, start=False, stop=True)
            P.act(MAT[ci][:], q[1][:, :], AF.Exp)
            P.mm(q[4][:, :], mk[:, cs], mq[:, cs])
            P.tt(Bm[ci][:], q[4][:, :], MAT[ci][:], ALU.mult)
            P.mm(q[6][:, 0:64], mk[:, cs], ident[0:64, 0:64])
            P.ts(kd[ci][:, 0:64], q[6][:, 0:64], c_kw, ALU.mult)
            P.mm(C.bankT[2][:, 0:129], Bm[ci][:], mva[:, j, :])
            P.mm(C.bankT[3][:, 0:129], mq[:, cs], S_old[:])
            P.mm(q[1][0:64, :], kd[ci][:, 0:64], mva[:, j, 0:128])
            P.mm(q[2][0:64, 0:1], kd[ci][:, 0:64], mva[:, j, 128:129])
            P.stt(S_new[:, 0:128], S_old[:, 0:128], c_gl[0:64, :], q[1][0:64, :], ALU.mult, ALU.add)
            P.stt(S_new[:, 128:129], S_old[:, 128:129], c_gl[0:64, :], q[2][0:64, 0:1], ALU.mult, ALU.add)
            P.act(osb[ci][:, 0:129], C.bankT[3][:, 0:129], AF.Identity, scale=c_e)
            P.tt(osb[ci][:, 0:129], osb[ci][:, 0:129], C.bankT[2][:, 0:129], ALU.add)
            P.act(ssq[ci][:, 0:1], osb[ci][:, 128:129], AF.Abs)
            P.ts(ssq[ci][:, 0:1], ssq[ci][:, 0:1], 1.0, ALU.max)
            P.recip(ssq[ci][:, 1:2], ssq[ci][:, 0:1])
            P.act(vnew[ci][:, 0:128], osb[ci][:, 0:128], AF.Identity, scale=ssq[ci][:, 1:2])
            P.act(ysb[ci][:, 0:128], vnew[ci][:, 0:128], AF.Square, accum=ssq[ci][:, 2:3])
            P.act(ssq[ci][:, 3:4], ssq[ci][:, 2:3], AF.Ln, bias=128.0 * 1e-6)
            P.act(ssq[ci][:, 3:4], ssq[ci][:, 3:4], AF.Exp, scale=-0.5)
            P.tt(ngz[ci][:, 0:128], mo[:, j, :], ng_ml, ALU.mult, eng="dve")
            P.stt(ysb[ci][:, 0:128], vnew[ci][:, 0:128], ssq[ci][:, 3:4], ngz[ci][:, 0:128], ALU.mult, ALU.mult)
            P.mm(q[3][:, :], ysb[ci][:, 0:128], ident)
            P.copy(ystm[:, cs], q[3][:, :], eng="act")
        P.dma(dr["yT"][128:256, t0:t0 + SEG], ystm[:])

        LA = P.end_defer()
        P.begin_defer()
        def shift(dst, z, mucol, npart):
            P.tt(tmpd[0:npart, :], z[0:npart, 0:SEG], z[0:npart, 1:SEG + 1], ALU.subtract)
            P.stt(dst[0:npart, :], tmpd[0:npart, :], mucol, z[0:npart, 1:SEG + 1], ALU.mult, ALU.add)
        shift(xw, zxw, colp[0:64, 36:37], 64)
        shift(xa, zxa, colp[0:64, 37:38], 64)
        shift(xg, zxg, colp[:, 38:39], 128)
        if has_vres:
            shift(xvt, zxv, colp[0:32, 39:40], 32)
        P.act(xw[:], xw[:], AF.Tanh)
        P.act(xg[:], xg[:], AF.Sigmoid)
        for h in range(2):
            cb = 12 + h * 12
            cp = lambda x_: colp[0:64, cb + x_:cb + x_ + 1]
            hs = slice(h * 64, (h + 1) * 64)
            shift(rr, zr[h], cp(0), 64)
            shift(rk, zk[h], cp(1), 64)
            shift(rvv, zv[h], cp(2), 64)
            P.mm(full[0][0:64, 0:SEG], w2s[:, hs], xw[:])
            P.act(logw[:], full[0][0:64, 0:SEG], AF.Sigmoid, bias=cp(3))
            P.ts(logw[:], logw[:], -float(np.exp(-0.5)), ALU.mult)
            P.mm(full[1][0:64, 0:SEG], a2s[:, hs], xa[:])
            P.act(av[:], full[1][0:64, 0:SEG], AF.Sigmoid, bias=cp(4))
            if has_vres:
                P.dma(vfs[:], dr["vfirst_in"][h * 64:(h + 1) * 64, t0:t0 + SEG])
                P.mm(full[0][0:64, 0:SEG], v2s[:, hs], xvt[:])
                P.act(tmpd[0:64, :], full[0][0:64, 0:SEG], AF.Sigmoid, bias=cp(8))
                P.tt(vfs[:], vfs[:], rvv[:], ALU.subtract)
                P.tt(vfs[:], vfs[:], tmpd[0:64, :], ALU.mult)
                P.tt(rvv[:], rvv[:], vfs[:], ALU.add)
            else:
                P.dma(dr["vfirst_out"][h * 64:(h + 1) * 64, t0:t0 + SEG], rvv[:])
            P.ts(kkv[:], rk[:], cp(5), ALU.mult)
            P.act(tmpd[0:64, :], kkv[:], AF.Square)
            P.mm(full[0][0:64, 0:SEG], ones[0:64, 0:64], tmpd[0:64, :])
            P.act(tmpd[0:64, :], full[0][0:64, 0:SEG], AF.Ln, bias=1e-6)
            P.act(tmpd[0:64, :], tmpd[0:64, :], AF.Exp, scale=-0.5)
            P.tt(kkv[:], kkv[:], tmpd[0:64, :], ALU.mult)
            P.ts(tmpd[0:64, :], av[:], -1.0, ALU.add, cp(6), ALU.mult)
            P.stt(k2v[:], tmpd[0:64, :], 1.0, rk[:], ALU.add, ALU.mult)
            P.stt(prod[:], rr[:], cp(7), k2v[:], ALU.mult, ALU.mult)
            P.scan(lw[:], rmR[:], logw[:], 0.0, ALU.mult, ALU.add)
            P.act(E1[:], lw[:], AF.Exp)
            P.act(E2[:], lw[:], AF.Exp, scale=-1.0)
            P.tt(tmpd[0:64, :], lw[:], logw[:], ALU.subtract)
            P.act(E3[:], tmpd[0:64, :], AF.Exp)
            cview = lambda t_: t_.re("p (n c) -> p n c", c=CR)
            lwl = cview(lw[:])[:, :, CR - 1:CR]
            P.tt(cview(tmpd[0:64, :]), cview(lw[:]), lwl.bt([64, NCR, CR]), ALU.subtract)
            P.act(tend[:], tmpd[0:64, :], AF.Exp, scale=-1.0)
            ARv = AR[:]
            P.stt(ARv[:, :, 0, :], cview(kkv[:]), -1.0, cview(E3[:]), ALU.mult, ALU.mult)
            P.tt(ARv[:, :, 1, :], cview(rr[:]), cview(E1[:]), ALU.mult)
            P.tt(tmpd[0:64, :], kkv[:], av[:], ALU.mult)
            P.tt(behat[:], tmpd[0:64, :], E2[:], ALU.mult)
            P.tt(beend[:], tmpd[0:64, :], tend[:], ALU.mult)
            P.tt(khat[:], k2v[:], E2[:], ALU.mult)
            P.tt(kend[:], k2v[:], tend[:], ALU.mult)
            lg_b = bc[0:CR, 256 + 128 * h:320 + 128 * h]
            lb_b = bc[0:CR, 320 + 128 * h:384 + 128 * h]
            for j in range(NCR):
                ci = j % 2
                cs = slice(j * CR, (j + 1) * CR)
                S_old, S_new = Sr[h][sri[h]], Sr[h][1 - sri[h]]
                sri[h] = 1 - sri[h]
                albar = ARv[:, j, 0, :]
                rbar = ARv[:, j, 1, :]
                arv = ARv[:, j, :, :]
                P.mm(half[0][0:CR, 0:2 * CR], behat[:, cs], arv)
                P.mm(half[1][0:CR, 0:2 * CR], khat[:, cs], arv)
                P.mm(C.r[0][0:CR, 0:CR], albar, behat[:, cs])
                gm = GM[ci]
                mS, mI = C.mulT_s[0:CR, 0:CR], C.mulT_i[0:CR, 0:CR]
                P.tt(gm[0:CR, 0:CR], half[0][0:CR, 0:CR], mS, ALU.mult)
                P.tt(gm[0:CR, CR:2 * CR], half[0][0:CR, CR:2 * CR], mI, ALU.mult)
                P.tt(gm[0:CR, 2 * CR:3 * CR], half[1][0:CR, 0:CR], mS, ALU.mult)
                P.tt(gm[0:CR, 3 * CR:4 * CR], half[1][0:CR, CR:2 * CR], mI, ALU.mult)
                P.tt(Am_r[ci][0:CR, 0:CR], C.r[0][0:CR, 0:CR], C.mulD_s[0:CR, 0:CR], ALU.mult)
                for x_, src in enumerate((rvv[:, cs], albar, beend[:, cs], kend[:, cs])):
                    P.mm(full[0][0:CR, x_ * 64:(x_ + 1) * 64], src, ident[0:64, 0:64])
                tk = TK[ci]
                P.copy(tk[0:CR, :], full[0][0:CR, 0:256], eng="act")
                v_t, al_t, be_t, ke_t = (tk[0:CR, x_ * 64:(x_ + 1) * 64] for x_ in range(4))
                R = tri_inverse(P, C, gm[0:CR, 0:CR], Am_r[ci][0:CR, 0:CR], CR, True, X_r, XT_r, Pm_r, PTm_r, C.inv_ps_r)
                P.mm(C.r[3][0:64, 0:CR], al_t, R)
                P.copy(wu[ci][:, 0:CR], C.r[3][0:64, 0:CR], eng="act")
                P.mm(C.r[4][0:CR, 0:64], gm[0:CR, 2 * CR:3 * CR], v_t)
                P.copy(lakv[ci][0:CR, :], C.r[4][0:CR, 0:64], eng="dve")
                P.mm(C.r[5][0:CR, 0:64], R, lakv[ci][0:CR, :], start=True, stop=False)
                P.mm(C.r[5][0:CR, 0:64], wu[ci][:, 0:CR], S_old[:], start=False, stop=True)
                P.copy(usb[ci][0:CR, :], C.r[5][0:CR, 0:64], eng="act")
                P.mm(C.r[7][0:CR, 0:64], rbar, S_old[:], start=True, stop=False)
                P.mm(C.r[7][0:CR, 0:64], gm[0:CR, CR:2 * CR], usb[ci][0:CR, :], start=False, stop=False)
                P.mm(C.r[7][0:CR, 0:64], gm[0:CR, 3 * CR:4 * CR], v_t, start=False, stop=True)
                P.mm(C.r[9][0:64, 0:64], be_t, usb[ci][0:CR, :], start=True, stop=False)
                P.mm(C.r[9][0:64, 0:64], ke_t, v_t, start=False, stop=True)
                P.stt(S_new[:], S_old[:], E1[:, j * CR + CR - 1:j * CR + CR], C.r[9][0:64, 0:64], ALU.mult, ALU.add)
                ob = osb_r[ci]
                P.copy(ob[0:CR, 0:64], C.r[7][0:CR, 0:64], eng="act")
                sq_ = ssq_r[ci]
                P.reduce(sq_[0:CR, 0:1], ob[0:CR, 0:64], ALU.add)
                P.ts(sq_[0:CR, 1:2], sq_[0:CR, 0:1], -1.0 / 64.0, ALU.mult)
                P.act(ob[0:CR, 64:128], ob[0:CR, 0:64], AF.Identity, bias=sq_[0:CR, 1:2])
                P.act(ysb_r[ci][0:CR, 0:64], ob[0:CR, 64:128], AF.Square, accum=sq_[0:CR, 2:3])
                P.act(sq_[0:CR, 3:4], sq_[0:CR, 2:3], AF.Ln, bias=64.0 * 64e-5)
                P.act(sq_[0:CR, 3:4], sq_[0:CR, 3:4], AF.Exp, scale=-0.5)
                P.stt(ysb_r[ci][0:CR, 0:64], ob[0:CR, 64:128], sq_[0:CR, 3:4], lg_b, ALU.mult, ALU.mult)
                P.tt(ysb_r[ci][0:CR, 0:64], ysb_r[ci][0:CR, 0:64], lb_b, ALU.add)
                P.mm(C.r[0][0:CR, 0:1], prod[:, cs], ones[0:64, 0:1])
                P.copy(cols_r[ci][0:CR, 0:1], C.r[0][0:CR, 0:1], eng="act")
                P.stt(ysb_r[ci][0:CR, 64:128], v_t, cols_r[ci][0:CR, 0:1], ysb_r[ci][0:CR, 0:64], ALU.mult, ALU.add)
                P.mm(C.r[3][0:CR, 0:64], xg[:, cs], g2s[:, hs])
                P.tt(vnew_r[ci][0:CR, 0:64], ysb_r[ci][0:CR, 64:128], C.r[3][0:CR, 0:64], ALU.mult)
                P.mm(C.r[4][0:64, 0:CR], vnew_r[ci][0:CR, 0:64], ident[0:CR, 0:CR])
                P.copy(ystr[:, cs], C.r[4][0:64, 0:CR], eng="act")
            P.dma(dr["yT"][256 + 64 * h:320 + 64 * h, t0:t0 + SEG], ystr[:])
        LB = P.end_defer()
        P.merge([LA, LB])


GDN_OFF, ML_OFF, RW_OFF, GATE_OFF = 0, 2056, 3600, 5392


def prep_mixer_inputs(inp, l, hq):
    f = np.float32
    has_vres = l > 0
    w_in = inp["w_in"][l]
    NF = NF0 + (32 if has_vres else 0)
    wfm = np.zeros((DM, NF), f)
    g0 = GDN_OFF
    wfm[:, F_GQ:F_GQ + 128] = w_in[:, g0 + hq * 128:g0 + (hq + 1) * 128]
    wfm[:, F_GK:F_GK + 128] = w_in[:, g0 + 512 + hq * 128:g0 + 512 + (hq + 1) * 128]
    wfm[:, F_GV:F_GV + 128] = w_in[:, g0 + 1024 + hq * 128:g0 + 1024 + (hq + 1) * 128]
    wfm[:, F_GG] = w_in[:, g0 + 2048 + hq]
    wfm[:, F_GG + 32] = w_in[:, g0 + 2052 + hq]
    m0 = ML_OFF
    wfm[:, F_MQ:F_MQ + 64] = w_in[:, m0 + hq * 64:m0 + (hq + 1) * 64]
    wfm[:, F_MK:F_MK + 64] = w_in[:, m0 + 256 + hq * 64:m0 + 256 + (hq + 1) * 64]
    wfm[:, F_MG] = w_in[:, m0 + 1536 + hq]
    wfm[:, F_MG + 32] = w_in[:, m0 + 1540 + hq]
    r0 = RW_OFF
    for h in range(2):
        hh = 2 * hq + h
        for x_ in range(3):
            wfm[:, F_RW + h * 192 + x_ * 64:F_RW + h * 192 + (x_ + 1) * 64] = w_in[:, r0 + x_ * 512 + hh * 64:r0 + x_ * 512 + (hh + 1) * 64]
    wfm[:, F_XW:F_XW + 64] = w_in[:, r0 + 1536:r0 + 1600]
    wfm[:, F_XA:F_XA + 64] = w_in[:, r0 + 1600:r0 + 1664]
    wfm[:, F_XG:F_XG + 128] = w_in[:, r0 + 1664:r0 + 1792]
    if has_vres:
        wfm[:, F_XV:F_XV + 32] = inp["rwkv_v1"][l - 1]
    wtm = np.zeros((DM, 384), f)
    wtm[:, 0:128] = w_in[:, g0 + 1536 + hq * 128:g0 + 1536 + (hq + 1) * 128]
    wtm[:, 128:256] = w_in[:, m0 + 512 + hq * 128:m0 + 512 + (hq + 1) * 128]
    wtm[:, 256:384] = w_in[:, m0 + 1024 + hq * 128:m0 + 1024 + (hq + 1) * 128]
    colp = np.zeros((128, NCOLP), f)
    conv = inp["gdn_conv"][l]
    for x_ in range(3):
        colp[:, x_ * 4:(x_ + 1) * 4] = conv[:, x_ * 512 + hq * 128:x_ * 512 + (hq + 1) * 128].T
    mu = inp["rwkv_mu"][l]
    for h in range(2):
        hh = 2 * hq + h
        cb = 12 + h * 12
        sl = slice(hh * 64, (hh + 1) * 64)
        colp[0:64, cb + 0] = mu[0:512][sl]
        colp[0:64, cb + 1] = mu[512:1024][sl]
        colp[0:64, cb + 2] = mu[1024:1536][sl]
        colp[0:64, cb + 3] = inp["rwkv_w0"][l][sl]
        colp[0:64, cb + 4] = inp["rwkv_a0"][l][sl]
        colp[0:64, cb + 5] = inp["rwkv_k_k"][l][sl]
        colp[0:64, cb + 6] = inp["rwkv_k_a"][l][sl]
        colp[0:64, cb + 7] = inp["rwkv_r_k"][l].reshape(-1)[sl]
        if has_vres:
            colp[0:64, cb + 8] = inp["rwkv_v0"][l - 1][sl]
    colp[0:64, 36] = mu[1536:1600]
    colp[0:64, 37] = mu[1600:1664]
    colp[:, 38] = mu[1664:1792]
    if has_vres:
        colp[0:32, 39] = inp["rwkv_mu_v1"][l - 1]
    rowp = np.array([[inp["gdn_dt_bias"][l][hq], inp["gdn_a_log"][l][hq], inp["mlstm_b_i"][l][hq], inp["mlstm_b_f"][l][hq]]], f)
    bc = np.zeros((1, 512), f)
    bc[0, 0:128] = inp["gdn_norm_g"][l]
    bc[0, 128:256] = inp["mlstm_norm_g"][l][hq * 128:(hq + 1) * 128]
    for h in range(2):
        hh = 2 * hq + h
        bc[0, 256 + 128 * h:320 + 128 * h] = inp["rwkv_lnx_g"][l][hh * 64:(hh + 1) * 64]
        bc[0, 320 + 128 * h:384 + 128 * h] = inp["rwkv_lnx_b"][l][hh * 64:(hh + 1) * 64]
    lora = np.zeros((128, 512), f)
    cs = slice(hq * 128, (hq + 1) * 128)
    lora[0:64, 0:128] = inp["rwkv_w2"][l][:, cs]
    lora[0:64, 128:256] = inp["rwkv_a2"][l][:, cs]
    lora[:, 256:384] = inp["rwkv_g2"][l][:, cs]
    if has_vres:
        lora[0:32, 384:512] = inp["rwkv_v2"][l - 1][:, cs]
    return dict(wfm=wfm, wtm=wtm, colp=colp, rowp=rowp, bc=bc, lora=lora)


def declare_mixer_dram(P, T, has_vres, pre, ext=True):
    NF = NF0 + (32 if has_vres else 0)
    kin = "ExternalInput" if ext else "Internal"
    dr = {}
    for nm, shp in (("wfm", [DM, NF]), ("wtm", [DM, 384]), ("colp", [128, NCOLP]), ("rowp", [1, 4]), ("bc", [1, 512]), ("lora", [128, 512])):
        dr[nm] = P.dram(f"{pre}_{nm}", shp, F32, kind="ExternalInput")
    return dr


MM_DT = BF16
ALPHA = float((2 * 2) ** 0.25)
LN_EPS = 1e-5


def mod_compute(P, C, dr, scratch, pre, need_gt=True, chunks=(0, 1, 2, 3, 4, 5)):
    sb = lambda shape, nm: P.sb(shape, name=f"{pre}m_{nm}")
    ccol = sb([128, 8], "ccol")
    P.dma(ccol[:], dr["ccol"][:])
    cond = sb([128, 8], "cond")
    P.act(cond[:], ccol[:], AF.Silu)
    condB = sb([128, 8, 128], "condB")
    for k in range(8):
        P.copy(condB[:, k, :], cond[:, k:k + 1].bt([128, 128]), eng="dve")
    abc = sb([128, 48], "abc")
    P.dma(abc[:], dr["adab_col"][:])
    modc = sb([128, 48], "modc")
    gtb = [sb([128, 1024], f"gtb{i}") for i in range(2)] if need_gt else None
    for i, ch in enumerate((2, 5) if need_gt else ()):
        P.dma(gtb[i][:], dr["adab_row"][:, ch * 1024:(ch + 1) * 1024].pb(128))
    BW = 256
    blk = scratch[:, 0:8 * BW].re("p (k f) -> p k f", k=8)
    pc = C.full[0]
    for ch in chunks:
        if ch in (2, 5) and not need_gt:
            continue
        for hf in range(1024 // BW):
            c0 = ch * 1024 + hf * BW
            P.dma(blk, dr["ada_w"][:, c0:c0 + BW].re("(k p) f -> p k f", p=128))
            if ch in (2, 5):
                gi = 0 if ch == 2 else 1
                for k in range(8):
                    P.mm(C.full[1][:, 0:BW], condB[:, k, :], blk[:, k, :], start=(k == 0), stop=(k == 7))
                P.tt(gtb[gi][:, hf * BW:(hf + 1) * BW], gtb[gi][:, hf * BW:(hf + 1) * BW], C.full[1][:, 0:BW], ALU.add)
            else:
                nj = BW // 128
                for j in range(nj):
                    for k in range(8):
                        P.mm(pc[:, j:j + 1], blk[:, k, j * 128:(j + 1) * 128], cond[:, k:k + 1], start=(k == 0), stop=(k == 7))
                cc = ch * 8 + hf * nj
                P.tt(modc[:, cc:cc + nj], pc[:, 0:nj], abc[:, cc:cc + nj], ALU.add)
    for i in range(2 if need_gt else 0):
        P.ts(gtb[i][:], gtb[i][:], 1.0, ALU.add)
    return modc, gtb


def ln_tile(P, xin, xout, gB, bB, st):
    P.reduce(st[:, 0:1], xin, ALU.add)
    P.ts(st[:, 1:2], st[:, 0:1], -1.0 / 1024.0, ALU.mult)
    P.act(xout, xin, AF.Identity, bias=st[:, 1:2])
    P.act(xin, xout, AF.Square, accum=st[:, 2:3])
    P.act(st[:, 3:4], st[:, 2:3], AF.Ln, scale=1.0 / 1024.0, bias=LN_EPS)
    P.act(st[:, 3:4], st[:, 3:4], AF.Exp, scale=-0.5)
    P.stt(xout, xout, st[:, 3:4], gB, ALU.mult, ALU.mult)
    P.tt(xout, xout, bB, ALU.add)


def to_feature_major(P, C, x_tm, hT_dst, sccol, shcol, bank):
    for k in range(8):
        reg = bank[:, (k % 4) * 128:(k % 4 + 1) * 128]
        P.mm(reg, x_tm[:, k * 128:(k + 1) * 128], C.ident)
        P.act(hT_dst[:, k, :], reg, AF.Identity, scale=sccol[:, k:k + 1], bias=shcol[:, k:k + 1])


def prologue_phase(P, C, NTOK, dr, pre="a"):
    sb = lambda shape, nm: P.sb(shape, name=f"{pre}s_{nm}")
    scratch = sb([128, 4096], "scr")
    modc, gtb = mod_compute(P, C, dr, scratch, pre, need_gt=False, chunks=(0, 1))
    sc1 = sb([128, 8], "sc1")
    P.ts(sc1[:], modc[:, 8:16], 1.0, ALU.add)
    gB, bB = sb([128, 1024], "gB"), sb([128, 1024], "bB")
    P.dma(gB[:], dr["lng"][:].pb(128))
    P.dma(bB[:], dr["lnb"][:].pb(128))
    xt = [sb([128, 1024], f"xt{i}") for i in range(2)]
    xo = [sb([128, 1024], f"xo{i}") for i in range(2)]
    hTs = [sb([128, 8, 128], f"hTs{i}") for i in range(2)]
    st = [sb([128, 4], f"st{i}") for i in range(2)]
    for t in range(NTOK // 128):
        i = t % 2
        P.dma(xt[i][:], dr["x"][t * 128:(t + 1) * 128, :])
        ln_tile(P, xt[i][:], xo[i][:], gB[:], bB[:], st[i])
        P.dma(dr["x0"][t * 128:(t + 1) * 128, :], xo[i][:])
        to_feature_major(P, C, xo[i], hTs[i], sc1, modc[:, 0:8], C.full[t % 2])
        P.dma(dr["hT"][:, t * 128:(t + 1) * 128].re("(k p) t -> p k t", p=128), hTs[i][:])


def post_phase(P, C, NTOK, TB, dr, kind, NFT, NE, emit_next, pre="c", gath=False):
    sb = lambda shape, nm: P.sb(shape, name=f"{pre}s_{nm}")
    NT = TB // 128
    ident = C.ident
    hid = P.sb([128, NFT, TB], MM_DT, name=f"{pre}s_hid")
    scr = sb([128, 2048], "scr")[:]
    modc, gtb = mod_compute(P, C, dr, scr, pre, chunks=(2, 3, 4, 5))
    sc2 = sb([128, 8], "sc2")
    P.ts(sc2[:], modc[:, 32:40], 1.0, ALU.add)
    sh2 = modc[:, 24:32]
    if emit_next:
        nx = {k[3:]: v for k, v in dr.items() if k.startswith("nx_")}
        modn, _ = mod_compute(P, C, nx, scr, pre + "n", need_gt=False, chunks=(0, 1))
        sc1n = sb([128, 8], "sc1n")
        P.ts(sc1n[:], modn[:, 8:16], 1.0, ALU.add)
    lnr = [sb([128, 1024], f"lnr{i}") for i in range(4)]
    for i, nm in enumerate(("ln1g", "ln1b", "ln2g", "ln2b")):
        P.dma(lnr[i][:], dr[nm][:].pb(128))
    LP = MM_DT != F32
    sig, tmp = sb([128, TB], "sig"), sb([128, TB], "tmp")
    sg, tmp2 = sb([128, TB], "sg"), sb([128, 1024], "tmp2")
    sbm = lambda shape, nm: P.sb(shape, MM_DT, name=f"{pre}s_{nm}")
    cast_i = [0]

    def cast(dst, src):
        cast_i[0] += 1
        P.copy(dst, src, eng=("dve" if cast_i[0] % 3 else "act"))
    wo = sbm([128, 8, 1024], "wo")
    if LP:
        for k in range(8):
            P.dma(tmp2[:], dr["wo"][k * 128:(k + 1) * 128, :])
            cast(wo[:, k, :], tmp2[:])
    else:
        P.dma(wo[:], dr["wo"][:].re("(k p) f -> p k f", p=128))
    hT32, yT32 = sb([128, 8, TB], "hT"), sb([128, 12, TB], "yT")
    hT, yT = (sbm([128, 8, TB], "hTb"), sbm([128, 12, TB], "yTb")) if LP else (hT32, yT32)
    mg = sbm([128, 8, TB], "mg")
    xr, x1 = sb([128, NT, 1024], "xr"), sb([128, NT, 1024], "x1")
    h2T32 = sb([128, 8, TB], "h2T") if (kind == "moe" or not LP) else None
    h2T = sbm([128, 8, TB], "h2Tb") if LP else h2T32
    wgj32s, wbj32 = [sb([128, 8, 128], f"wgj{i}") for i in range(2)], sb([128, 12, 128], "wbj")
    wgj32 = wgj32s[0]
    wgj, wbj = (sbm([128, 8, 128], "wgjb"), sbm([128, 12, 128], "wbjb")) if LP else (wgj32, wbj32)
    NBUF = 2 if LP else 3
    wgu32 = [sb([128, 2, 8, 128], f"wgu{i}") for i in range(NBUF)]
    wd32 = [sb([128, 1024], f"wd{i}") for i in range(NBUF)]
    wgu = [sbm([128, 2, 8, 128], f"wgub{i}") for i in range(2)] if LP else wgu32
    wd = [sbm([128, 1024], f"wdb{i}") for i in range(2)] if LP else wd32
    st = sb([128, 8], "st")
    yacc = sb([128, NT, 1024], "yacc")
    if kind == "moe":
        rt = sb([128, 8, 8], "rt")
        P.dma(rt[:], dr["router"][:].re("(k p) e -> p k e", p=128))
        rb = sb([128, 8], "rb")
        P.dma(rb[:], dr["router_b"][:].pb(128))
        lg, eq1, lg2, eq2, gts = (sb([128, NT, 8], n_) for n_ in ("lg", "eq1", "lg2", "eq2", "gts"))
        mst = sb([128, NT, 8], "mst")
    self_banks = C.bankT[:2 * NT]
    if gath:
        yg, yq = dr["yT"], dr["yTq"]
        ygf = yg.t.ap().rearrange("c i r w -> (c i r) w")
        for i in range(4):
            def dyn(val, i=i):
                return ygf[i * 384:(i + 1) * 384, bass.ds(val * NTOK, NTOK)]
            P.dma(yq[i * 384:(i + 1) * 384, :], V(ygf[i * 384:(i + 1) * 384, 0:NTOK], [yg.tok]), dyn=dyn)
    for b0 in range(NTOK // TB):
        ts_ = slice(b0 * TB, (b0 + 1) * TB)
        P.dma(hT32[:], dr["hT"][:, ts_].re("(k p) t -> p k t", p=128))
        if LP:
            cast(hT[:], hT32[:])
        if not gath:
            P.dma(yT32[:], dr["yT"][:, ts_].re("(k p) t -> p k t", p=128))
            if LP:
                cast(yT[:], yT32[:])
        else:
            yq = dr["yTq"]
            for r in range(3):
                for pc in range(4):
                    c_ = r * 4 + pc
                    P.dma(yT32[pc * 32:(pc + 1) * 32, r * 4:(r + 1) * 4, :], yq[c_ * 128:(c_ + 1) * 128, ts_].re("(i rr) t -> rr i t", i=4))
            if LP:
                cast(yT[:], yT32[:])
        for t in range(NT):
            P.dma(xr[:, t, :], dr["x"][b0 * TB + t * 128:b0 * TB + (t + 1) * 128, :])
        for j in range(8):
            P.dma(wbj32[:], dr["wbj"][j])
            if LP:
                cast(wbj[:], wbj32[:])
            for r in range(3):
                pg, pp = C.full[0], C.full[1]
                wgj32 = wgj32s[(j * 3 + r) % 2]
                P.dma(wgj32[:], dr["wgj"][j][:, r, :, :])
                if LP:
                    cast(wgj[:], wgj32[:])
                else:
                    wgj = wgj32
                for k in range(8):
                    P.mm(pg[:, 0:TB], wgj[:, k, :], hT[:, k, :], start=(k == 0), stop=(k == 7))
                P.act(sig[:], pg[:, 0:TB], AF.Sigmoid)
                for k in range(4):
                    P.mm(pp[:, 0:TB], wbj[:, r * 4 + k, :], yT[:, r * 4 + k, :], start=(k == 0), stop=(k == 3))
                if r == 0:
                    P.tt(mg[:, j, :], pp[:, 0:TB], sig[:], ALU.mult)
                else:
                    P.tt(tmp[:], pp[:, 0:TB], sig[:], ALU.mult)
                    P.tt(mg[:, j, :], mg[:, j, :], tmp[:], ALU.add)
        for t in range(NT):
            for hf in range(2):
                po = C.full[hf]
                for k in range(8):
                    P.mm(po[:, 0:512], mg[:, k, t * 128:(t + 1) * 128], wo[:, k, hf * 512:(hf + 1) * 512], start=(k == 0), stop=(k == 7))
                P.tt(tmp2[:, hf * 512:(hf + 1) * 512], po[:, 0:512], gtb[0][:, hf * 512:(hf + 1) * 512], ALU.mult)
            P.stt(tmp2[:], xr[:, t, :], ALPHA, tmp2[:], ALU.mult, ALU.add)
            ln_tile(P, tmp2[:], x1[:, t, :], lnr[0][:], lnr[1][:], st)
            for k in range(8):
                reg = C.q[(k % 4) * 4][:, :]
                P.mm(reg, x1[:, t, k * 128:(k + 1) * 128], ident)
                if h2T32 is not None and LP:
                    P.act(h2T32[:, k, t * 128:(t + 1) * 128], reg, AF.Identity, scale=sc2[:, k:k + 1], bias=sh2[:, k:k + 1])
                    P.copy(h2T[:, k, t * 128:(t + 1) * 128], h2T32[:, k, t * 128:(t + 1) * 128], eng="dve")
                else:
                    P.act(h2T[:, k, t * 128:(t + 1) * 128], reg, AF.Identity, scale=sc2[:, k:k + 1], bias=sh2[:, k:k + 1])

        def swiglu(wgu_d, wd_d, consume):
            for i in range(NFT):
                w_ = wgu[i % 2] if LP else wgu32[i % NBUF]
                P.dma(wgu32[i % NBUF][:], wgu_d[i])
                if LP:
                    cast(w_[:], wgu32[i % NBUF][:])
                pg, pu = C.full[0], C.full[1]
                for k in range(8):
                    P.mm(pg[:, 0:TB], w_[:, 0, k, :], h2T[:, k, :], start=(k == 0), stop=(k == 7))
                P.act(sg[:], pg[:, 0:TB], AF.Silu)
                for k in range(8):
                    P.mm(pu[:, 0:TB], w_[:, 1, k, :], h2T[:, k, :], start=(k == 0), stop=(k == 7))
                P.tt(hid[:, i, :], pu[:, 0:TB], sg[:], ALU.mult)
            for i in range(NFT):
                w2 = wd[i % 2] if LP else wd32[i % NBUF]
                P.dma(wd32[i % NBUF][:], wd_d[i * 128:(i + 1) * 128, :])
                if LP:
                    cast(w2[:], wd32[i % NBUF][:])
                for t in range(NT):
                    for hf in range(2):
                        P.mm(self_banks[2 * t + hf][:, 0:512], hid[:, i, t * 128:(t + 1) * 128], w2[:, hf * 512:(hf + 1) * 512],
                             start=(i == 0), stop=(i == NFT - 1))
            for t in range(NT):
                for hf in range(2):
                    consume(t, hf, self_banks[2 * t + hf][:, 0:512])

        if kind == "dense":
            def consume(t, hf, ps_):
                P.tt(yacc[:, t, hf * 512:(hf + 1) * 512], ps_, gtb[1][:, hf * 512:(hf + 1) * 512], ALU.mult)
            swiglu(dr["wgu"], dr["wd"], consume)
        else:
            for t in range(NT):
                pl = C.full[0]
                for k in range(8):
                    P.mm(pl[:, 0:8], h2T32[:, k, t * 128:(t + 1) * 128], rt[:, k, :], start=(k == 0), stop=(k == 7))
                P.tt(lg[:, t, :], pl[:, 0:8], rb[:], ALU.add)
                P.reduce(mst[:, t, 0:1], lg[:, t, :], ALU.max)
                P.ts(eq1[:, t, :], lg[:, t, :], mst[:, t, 0:1], ALU.is_equal)
                P.stt(lg2[:, t, :], eq1[:, t, :], -1e30, lg[:, t, :], ALU.mult, ALU.add)
                P.reduce(mst[:, t, 1:2], lg2[:, t, :], ALU.max)
                P.ts(eq2[:, t, :], lg2[:, t, :], mst[:, t, 1:2], ALU.is_equal)
                P.tt(mst[:, t, 2:3], mst[:, t, 1:2], mst[:, t, 0:1], ALU.subtract)
                P.act(mst[:, t, 3:4], mst[:, t, 2:3], AF.Exp)
                P.ts(mst[:, t, 4:5], mst[:, t, 3:4], 1.0, ALU.add)
                P.recip(mst[:, t, 5:6], mst[:, t, 4:5])
                P.tt(mst[:, t, 6:7], mst[:, t, 3:4], mst[:, t, 5:6], ALU.mult)
                P.ts(gts[:, t, :], eq1[:, t, :], mst[:, t, 5:6], ALU.mult)
                P.stt(gts[:, t, :], eq2[:, t, :], mst[:, t, 6:7], gts[:, t, :], ALU.mult, ALU.add)
            import os as _os
            for e in range(int(_os.environ.get('DBGNE', NE))):
                def consume(t, hf, ps_, e=e):
                    sl = yacc[:, t, hf * 512:(hf + 1) * 512]
                    if e == 0:
                        P.ts(sl, ps_, gts[:, t, e:e + 1], ALU.mult)
                    else:
                        P.ts(tmp2[:, 0:512], ps_, gts[:, t, e:e + 1], ALU.mult)
                        P.tt(sl, sl, tmp2[:, 0:512], ALU.add)
                swiglu(dr["wgu"][e], dr["wd"][e], consume)
            for t in range(NT):
                P.tt(yacc[:, t, :], yacc[:, t, :], gtb[1][:], ALU.mult)
        for t in range(NT):
            P.stt(tmp2[:], x1[:, t, :], ALPHA, yacc[:, t, :], ALU.mult, ALU.add)
            ln_tile(P, tmp2[:], xr[:, t, :], lnr[2][:], lnr[3][:], st)
            P.dma(dr["xo"][b0 * TB + t * 128:b0 * TB + (t + 1) * 128, :], xr[:, t, :])
            if emit_next:
                for k in range(8):
                    reg = C.q[(k % 4) * 4][:, :]
                    P.mm(reg, xr[:, t, k * 128:(k + 1) * 128], ident)
                    P.act(hT32[:, k, t * 128:(t + 1) * 128], reg, AF.Identity, scale=sc1n[:, k:k + 1], bias=modn[:, k:k + 1])
        if emit_next:
            P.dma(dr["hTn"][:, ts_].re("(k p) t -> p k t", p=128), hT32[:])


def prep_mod_inputs(inp, l, b):
    f = np.float32
    return dict(ccol=np.ascontiguousarray(inp["c"][b].reshape(8, 128).T.astype(f)),
                ada_w=inp["ada_w"][l],
                adab_col=np.ascontiguousarray(inp["ada_b"][l].reshape(48, 128).T.astype(f)),
                adab_row=inp["ada_b"][l].reshape(1, 6144))


def prep_post_weights(inp, l, NFT=None, NE=None):
    f = np.float32
    d = {}
    wg = inp["w_in"][l][:, GATE_OFF:GATE_OFF + 3072]
    d["wgj"] = np.ascontiguousarray(wg.reshape(8, 128, 3, 8, 128).transpose(3, 1, 2, 0, 4))
    wb = inp["w_branch"][l].reshape(12, 128, 8, 128)
    d["wbj"] = np.ascontiguousarray(wb.transpose(2, 1, 0, 3))
    d["wo"] = inp["w_o"][l]
    for i, nm in enumerate(("ln1_g", "ln1_b", "ln2_g", "ln2_b")):
        d[("ln1g", "ln1b", "ln2g", "ln2b")[i]] = inp[nm][l].reshape(1, 1024)

    def gu(wg_, wu_, nft):
        a = wg_[:, :nft * 128].reshape(8, 128, nft, 128).transpose(2, 1, 0, 3)
        b = wu_[:, :nft * 128].reshape(8, 128, nft, 128).transpose(2, 1, 0, 3)
        return np.ascontiguousarray(np.stack([a, b], axis=2))
    if l % 2 == 0:
        i = l // 2
        nft = NFT or 22
        d["wgu"] = gu(inp["ffn_w_gate"][i], inp["ffn_w_up"][i], nft)
        d["wd"] = np.ascontiguousarray(inp["ffn_w_down"][i][:nft * 128])
    else:
        i = l // 2
        nft = NFT or 28
        ne = NE or 8
        d["wgu"] = np.stack([gu(inp["moe_w_gate"][i][e], inp["moe_w_up"][i][e], nft) for e in range(ne)])
        d["wd"] = np.ascontiguousarray(inp["moe_w_down"][i][:ne, :nft * 128])
        d["router"] = np.ascontiguousarray(inp["moe_router"][i][:, :8])
        d["router_b"] = inp["moe_router_b"][i].reshape(1, 8)
    return d


def declare_inputs(P, arrays, pre):
    return {k: P.dram(f"{pre}_{k}", list(v.shape), F32, kind="ExternalInput") for k, v in arrays.items()}


NB, NQ = 2, 4
SEG = 256
TBLK = 256
GROUPS = [[0, 1, 2, 3], [4, 5, 6, 7]]


def _new_prog():
    nc = bass.Bass("TRN2", target_bir_lowering=False)
    P = Prog(nc)
    consts = P.dram("consts", [128, 896], F32, kind="ExternalInput")
    C = setup_common(P, consts)
    return nc, P, C


def fused_inputs(inp, TSEQ, nft_d=22, nft_m=28):
    NTOK = TSEQ // NQ
    consts = host_consts()
    pws = [prep_post_weights(inp, 0, nft_d), prep_post_weights(inp, 1, nft_m)]
    maps = []
    for b in range(NB):
        mods = [prep_mod_inputs(inp, l, b) for l in range(2)]
        for q in range(NQ):
            m = {"consts": consts, "qidx": np.array([[q]], np.int32)}
            m.update({"a_" + k: v for k, v in mods[0].items()})
            m["a_x"] = np.ascontiguousarray(inp["x"][b, q * NTOK:(q + 1) * NTOK])
            m["a_lng"] = inp["ln_in_g"].reshape(1, 1024)
            m["a_lnb"] = inp["ln_in_b"].reshape(1, 1024)
            for l in range(2):
                m.update({f"m{l}_" + k: v for k, v in prep_mixer_inputs(inp, l, q).items()})
                m.update({f"c{l}_" + k: v for k, v in pws[l].items()})
                m.update({f"c{l}_" + k: v for k, v in mods[l].items()})
            m.update({"c0_nx_" + k: v for k, v in mods[1].items()})
            maps.append(m)
    return maps


def build_fused(maps0, TSEQ, nft_d=22, nft_m=28):
    NTOK = TSEQ // NQ
    nc, P, C = _new_prog()
    ext = {k: P.dram(k, list(v.shape), mybir.dt.int32 if v.dtype == np.int32 else F32, kind="ExternalInput")
           for k, v in maps0.items() if k != "consts"}
    sub = lambda pre: {k[len(pre):]: v for k, v in ext.items() if k.startswith(pre)}
    out = P.dram("out", [NTOK, 1024], F32, kind="ExternalOutput")
    x0 = P.dram("i_x0", [NTOK, 1024])
    x1 = P.dram("i_x1", [NTOK, 1024])
    hTs = [P.dram(f"i_hTs{l}", [1024, NTOK]) for l in range(2)]
    hTg = [P.dram(f"i_hTg{l}", [8, 4, 128, NTOK]) for l in range(2)]
    yT = [P.dram(f"i_yT{l}", [384, TSEQ]) for l in range(2)]
    yTg = [P.dram(f"i_yTg{l}", [12, 4, 32, TSEQ]) for l in range(2)]
    vf = P.dram("i_vf", [128, TSEQ])
    yTq = [P.dram(f"i_yTq{l}", [4 * 384, NTOK]) for l in range(2)]
    P.load_dyn(ext["qidx"][0:1, 0:1])
    with P.scope():
        dr = sub("a_")
        dr.update(x0=x0, hT=hTs[0])
        prologue_phase(P, C, NTOK, dr)
    xin = x0
    import os as _os
    stop = int(_os.environ.get("FUSE_STOP", 99))
    for l in range(2):
        if stop <= 1 + 4 * l:
            break
        P.all_gather_rows(hTg[l], hTs[l], 128, GROUPS)
        if stop <= 2 + 4 * l:
            break
        with P.scope():
            dr = sub(f"m{l}_")
            dr.update(hT=hTg[l], yT=yT[l])
            dr["vfirst_in" if l else "vfirst_out"] = vf
            mixer_phase(P, C, TSEQ, SEG, dr, l > 0, pre=f"m{l}", gath=NTOK)
        if stop <= 3 + 4 * l:
            break
        P.all_gather_rows(yTg[l], yT[l], 32 if TSEQ * 32 * 4 <= (1 << 20) else 16, GROUPS)
        if stop <= 4 + 4 * l:
            break
        with P.scope():
            dr = sub(f"c{l}_")
            dr.update(x=xin, hT=hTs[l], yT=yTg[l], yTq=yTq[l], xo=(x1 if l == 0 else out))
            if l == 0:
                dr["hTn"] = hTs[1]
            post_phase(P, C, NTOK, TBLK, dr, "dense" if l == 0 else "moe", nft_d if l == 0 else nft_m, 8, l == 0, pre=f"c{l}", gath=True)
        xin = x1
    P.emit()
    return nc, P


def kernel(**inputs):
    inp = {k: np.asarray(v) for k, v in inputs.items()}
    TSEQ = inp["x"].shape[1]
    NTOK = TSEQ // NQ
    maps = fused_inputs(inp, TSEQ)
    nc, P = build_fused(maps[0], TSEQ)
    res = run_bass_kernel_spmd(nc, maps, core_ids=list(range(NB * NQ))).results
    out = np.zeros((NB, TSEQ, 1024), np.float32)
    for ci in range(NB * NQ):
        b, q = divmod(ci, NQ)
        out[b, q * NTOK:(q + 1) * NTOK] = res[ci]["out"]
    return out
```
